# Optimizing a Trainium2 kernel written in Bass

```python
import jax
import jax.numpy as jnp
from jax import lax

D_MODEL = 1024
BATCH = 8
SEQ = 2048
DEPTH = 1

CHUNK = 64
EPS = 1e-6
CONV_CH = D_MODEL
CONV_WIDTH = 31
SGU_CH = D_MODEL
SGU_GROUPS = 8
SGU_GROUP_CH = SGU_CH // SGU_GROUPS
SGU_BLOCK = 128
IN_WIDTH = 2 * CONV_CH + 2 * SGU_CH + 2 * D_MODEL
PEER_HEADS = 8
PEER_N_KEYS = 128
PEER_N_EXPERTS = PEER_N_KEYS * PEER_N_KEYS
PEER_TOPK = 16
PEER_D_KEY = 256
PEER_D_HALF = PEER_D_KEY // 2
PEER_TOKEN_BLOCK = 128

kernel_name = "hybrid_conv_sgu_peer_adaln_block"


def rms_norm(x, g):
    xf = x.astype(jnp.float32)
    y = xf * lax.rsqrt(jnp.mean(xf * xf, axis=-1, keepdims=True) + EPS)
    return (y * g.astype(jnp.float32)).astype(x.dtype)


def layer_norm(x, g, b):
    xf = x.astype(jnp.float32)
    mu = jnp.mean(xf, axis=-1, keepdims=True)
    var = jnp.mean(jnp.square(xf - mu), axis=-1, keepdims=True)
    y = (xf - mu) * lax.rsqrt(var + EPS)
    return (y * g.astype(jnp.float32) + b.astype(jnp.float32)).astype(x.dtype)


def modulate(n, shift, scale):
    return n * (1 + scale[:, None, :]) + shift[:, None, :]


def conv_branch(a_val, a_gate, dw_w, dw_b, ln_g, ln_b, w_proj):
    z = a_val * jax.nn.sigmoid(a_gate)
    z = lax.conv_general_dilated(
        z, dw_w[:, None, :], window_strides=(1,),
        padding=[(CONV_WIDTH - 1, 0)],
        dimension_numbers=("NWC", "WIO", "NWC"),
        feature_group_count=CONV_CH) + dw_b
    z = jax.nn.silu(layer_norm(z, ln_g, ln_b))
    return z @ w_proj


def sgu_branch(u, v, ln_g, ln_b, w_s, b_s, w_proj):
    B, S, _ = u.shape
    v = layer_norm(v, ln_g, ln_b)
    pos_chunk = jnp.arange(SGU_BLOCK) // CHUNK
    mask = pos_chunk[None, :] <= pos_chunk[:, None]
    w_masked = jnp.where(mask[None], w_s, 0)
    vb = v.reshape(B, S // SGU_BLOCK, SGU_BLOCK, SGU_GROUPS, SGU_GROUP_CH)
    mixed = jnp.einsum("gij,bnjgc->bnigc", w_masked, vb) + b_s.T[None, None, :, :, None]
    gated = u * mixed.reshape(B, S, SGU_CH)
    return gated @ w_proj


def peer(n, w_query, sub_keys, expert_u, expert_v):
    B, S, D = n.shape
    T = B * S
    xt = n.reshape(T, D)
    q = (xt @ w_query).reshape(T, PEER_HEADS, 2, PEER_D_HALF)
    scores = jnp.einsum("thpd,hpkd->thpk", q, sub_keys).astype(jnp.float32)
    s1, i1 = lax.top_k(scores[:, :, 0], PEER_TOPK)
    s2, i2 = lax.top_k(scores[:, :, 1], PEER_TOPK)
    cand = (s1[..., :, None] + s2[..., None, :]).reshape(T, PEER_HEADS, PEER_TOPK * PEER_TOPK)
    s, ci = lax.top_k(cand, PEER_TOPK)
    e_idx = (jnp.take_along_axis(i1, ci // PEER_TOPK, axis=-1) * PEER_N_KEYS
             + jnp.take_along_axis(i2, ci % PEER_TOPK, axis=-1))
    gate = jax.nn.softmax(s, axis=-1).astype(n.dtype)
    nb = T // PEER_TOKEN_BLOCK
    hk = PEER_HEADS * PEER_TOPK
    x_blk = xt.reshape(nb, PEER_TOKEN_BLOCK, D)
    i_blk = e_idx.reshape(nb, PEER_TOKEN_BLOCK, hk)
    g_blk = gate.reshape(nb, PEER_TOKEN_BLOCK, hk)

    def apply_experts(args):
        xb, ib, gb = args
        u = expert_u[ib]
        a = jnp.einsum("td,tkd->tk", xb, u)
        h = jax.nn.gelu(a) * gb
        return jnp.einsum("tk,tkd->td", h, expert_v[ib])

    y = lax.map(apply_experts, (x_blk, i_blk, g_blk))
    return y.reshape(B, S, D)


def setup_inputs(seed: int = 0) -> dict:
    key = jax.random.key(seed)
    ks = jax.random.split(key, 24)
    f32 = jnp.float32
    L, D = DEPTH, D_MODEL

    def nrm(k, shape, scale):
        return jax.random.normal(k, shape, f32) * scale

    return {
        "x": nrm(ks[0], (BATCH, SEQ, D), 1.0),
        "c": nrm(ks[1], (BATCH, D), 1.0),
        "w_ada": nrm(ks[2], (L, D, 6 * D), 0.5 * D ** -0.5),
        "b_ada": nrm(ks[3], (L, 6 * D), 0.01),
        "g_norm1": 1.0 + nrm(ks[4], (L, D), 0.02),
        "w_in": nrm(ks[5], (L, D, IN_WIDTH), D ** -0.5),
        "conv_dw_w": nrm(ks[6], (L, CONV_WIDTH, CONV_CH), CONV_WIDTH ** -0.5),
        "conv_dw_b": nrm(ks[7], (L, CONV_CH), 0.01),
        "conv_ln_g": 1.0 + nrm(ks[8], (L, CONV_CH), 0.02),
        "conv_ln_b": nrm(ks[9], (L, CONV_CH), 0.01),
        "w_conv_out": nrm(ks[10], (L, CONV_CH, D), CONV_CH ** -0.5),
        "sgu_ln_g": 1.0 + nrm(ks[11], (L, SGU_CH), 0.02),
        "sgu_ln_b": nrm(ks[12], (L, SGU_CH), 0.01),
        "w_spatial": nrm(ks[13], (L, SGU_GROUPS, SGU_BLOCK, SGU_BLOCK), SGU_BLOCK ** -0.5),
        "b_spatial": 1.0 + nrm(ks[14], (L, SGU_GROUPS, SGU_BLOCK), 0.02),
        "w_sgu_out": nrm(ks[15], (L, SGU_CH, D), SGU_CH ** -0.5),
        "w_out": nrm(ks[16], (L, D, D), D ** -0.5),
        "g_norm2": 1.0 + nrm(ks[17], (L, D), 0.02),
        "w_query": nrm(ks[18], (L, D, PEER_HEADS * PEER_D_KEY), D ** -0.5),
        "sub_keys": nrm(ks[19], (L, PEER_HEADS, 2, PEER_N_KEYS, PEER_D_HALF), PEER_D_HALF ** -0.5),
        "expert_u": nrm(ks[20], (L, PEER_N_EXPERTS, D), D ** -0.5),
        "expert_v": nrm(ks[21], (L, PEER_N_EXPERTS, D), 1.0),
        "g_final": 1.0 + nrm(ks[22], (D,), 0.02),
    }


def reference(x, c, w_ada, b_ada, g_norm1, w_in, conv_dw_w, conv_dw_b, conv_ln_g, conv_ln_b,
              w_conv_out, sgu_ln_g, sgu_ln_b, w_spatial, b_spatial, w_sgu_out, w_out,
              g_norm2, w_query, sub_keys, expert_u, expert_v, g_final):
    splits = [CONV_CH, 2 * CONV_CH, 2 * CONV_CH + SGU_CH, 2 * CONV_CH + 2 * SGU_CH,
              2 * CONV_CH + 2 * SGU_CH + D_MODEL]
    c_act = jax.nn.silu(c)
    h = x
    for l in range(DEPTH):
        mod = c_act @ w_ada[l] + b_ada[l]
        shift1, scale1, gate1, shift2, scale2, gate2 = jnp.split(mod, 6, axis=-1)

        n = modulate(rms_norm(h, g_norm1[l]), shift1, scale1)
        p = n @ w_in[l]
        a_val, a_gate, u, v, gate_a, gate_b = jnp.split(p, splits, axis=-1)
        y_a = conv_branch(a_val, a_gate, conv_dw_w[l], conv_dw_b[l], conv_ln_g[l], conv_ln_b[l],
                          w_conv_out[l])
        y_b = sgu_branch(u, v, sgu_ln_g[l], sgu_ln_b[l], w_spatial[l], b_spatial[l], w_sgu_out[l])
        merged = jax.nn.sigmoid(gate_a) * y_a + jax.nn.sigmoid(gate_b) * y_b
        h = h + gate1[:, None, :] * (merged @ w_out[l])

        n2 = modulate(rms_norm(h, g_norm2[l]), shift2, scale2)
        h = h + gate2[:, None, :] * peer(n2, w_query[l], sub_keys[l], expert_u[l], expert_v[l])
    return rms_norm(h, g_final)
```

```python
import math
from contextlib import ExitStack

import numpy as np
import concourse.bass as bass
import concourse.mybir as mybir
from concourse.bass_utils import run_bass_kernel_spmd

F32 = mybir.dt.float32
BF16 = mybir.dt.bfloat16
U32 = mybir.dt.uint32
I32 = mybir.dt.int32
AF = mybir.ActivationFunctionType
ALU = mybir.AluOpType
AX = mybir.AxisListType

D = 1024
S = 2048
NT = S // 128
TT = 256
NTT = S // TT
EPS = 1e-6
NSLOT = 128
RING = 16
BATCH = 4
GELU_C = 2.0 * math.sqrt(2.0 / math.pi)

_DSIZE = {F32: 4, BF16: 2, U32: 4, I32: 4}


class Arena:
    def __init__(self, base):
        self.base = base
        self.off = 0
        self.cap = base.shape[1]

    def alloc(self, free_shape, dtype):
        n = 1
        for d in free_shape:
            n *= d
        words = (n * _DSIZE[dtype] + 3) // 4
        words = (words + 1) // 2 * 2
        assert self.off + words <= self.cap, ("arena overflow", self.off, words, self.cap)
        ap = self.base[:, self.off:self.off + words]
        if dtype != F32:
            ap = ap.bitcast(dtype)
        ap = ap[:, 0:n]
        if len(free_shape) == 2:
            ap = ap.rearrange("p (a b) -> p a b", a=free_shape[0])
        elif len(free_shape) == 3:
            ap = ap.rearrange("p (a b c) -> p a b c", a=free_shape[0], b=free_shape[1])
        self.off += words
        return ap


class Prog:
    def __init__(self, nc, es):
        self.nc = nc
        self.es = es
        self.names = {"pe": "tensor", "act": "scalar", "dve": "vector", "pool": "gpsimd", "sp": "sync"}
        self.stream = {e: [] for e in self.names}
        self.esem = {e: es.enter_context(nc.semaphore("se_" + e)) for e in self.names}
        self.ecnt = {e: 0 for e in self.names}
        self.dsem = {}
        self.dcnt = {}
        self.lastw = {}
        self.readers = {}
        self.waited = {e: {} for e in self.names}

    def _need(self, e, tok, same_ok):
        semkey, val, src = tok
        if same_ok and src == e and e == "pe":
            return
        if semkey[0] == "d":
            val = self.dcnt[semkey[1]]
        if self.waited[e].get(semkey, 0) >= val:
            return
        self.waited[e][semkey] = val
        self.stream[e].append(("w", semkey, val))

    def _deps(self, e, reads, writes):
        for k in reads:
            t = self.lastw.get(k)
            if t is not None:
                self._need(e, t, same_ok=False)
        for k in writes:
            t = self.lastw.get(k)
            if t is not None:
                self._need(e, t, same_ok=True)
            for sk, (v, src) in self.readers.get(k, {}).items():
                self._need(e, (sk, v, src), same_ok=True)

    def _record(self, tok, reads, writes):
        for k in writes:
            self.lastw[k] = tok
            self.readers[k] = {}
        for k in reads:
            if k in writes:
                continue
            self.readers.setdefault(k, {})[tok[0]] = (tok[1], tok[2])

    def op(self, e, fn, reads=(), writes=()):
        self._deps(e, reads, writes)
        self.ecnt[e] += 1
        tok = (("e", e), self.ecnt[e], e)
        self.stream[e].append(("op", fn))
        self._record(tok, reads, writes)

    def dma(self, q, fn, sem, reads=(), writes=()):
        if sem not in self.dsem:
            self.dsem[sem] = self.es.enter_context(self.nc.semaphore("sd_" + sem))
            self.dcnt[sem] = 0
        self._deps(q, reads, writes)
        if self.dcnt[sem] > 0:
            self._need(q, (("d", sem), self.dcnt[sem], None), same_ok=False)
        self.dcnt[sem] += 16
        tok = (("d", sem), self.dcnt[sem], None)
        self.stream[q].append(("dma", fn, sem))
        self._record(tok, reads, writes)

    def barrier(self, skip_prefix=None):
        for e in self.names:
            for e2 in self.names:
                if e2 != e and self.ecnt[e2] > 0:
                    self._need(e, (("e", e2), self.ecnt[e2], e2), same_ok=False)
            for sname in self.dsem:
                if skip_prefix is not None and sname.startswith(skip_prefix):
                    continue
                if self.dcnt[sname] > 0:
                    self._need(e, (("d", sname), self.dcnt[sname], None), same_ok=False)

    def finish(self, sems):
        for sname in sems:
            self._need("sp", (("d", sname), self.dcnt[sname], None), same_ok=False)

    def _semobj(self, semkey):
        return self.esem[semkey[1]] if semkey[0] == "e" else self.dsem[semkey[1]]

    def emit(self):
        with self.nc.Block() as block:
            for e, nm in self.names.items():
                def body(E, e=e):
                    for item in self.stream[e]:
                        if item[0] == "w":
                            E.wait_ge(self._semobj(item[1]), item[2])
                        elif item[0] == "raw":
                            item[1](E)
                        elif item[0] == "op":
                            item[1](E).then_inc(self.esem[e], 1)
                        else:
                            item[1](E).then_inc(self.dsem[item[2]], 16)
                getattr(block, nm)(body)


def build_nc(debug=None):
    nc = bass.Bass("TRN2", target_bir_lowering=False)

    def din(name, shape, dt=F32):
        return nc.dram_tensor(name, list(shape), dt, kind="ExternalInput").ap()

    x = din("x", [S, D])
    c_t = din("c_t", [128, 8])
    w_ada = din("w_ada", [D, 6 * D])
    b_ada = din("b_ada", [1, 6 * D])
    g1 = din("g1", [1, D])
    g2 = din("g2", [1, D])
    gf = din("gf", [1, D])
    w_in = din("w_in", [D, 6 * D])
    w_co = din("w_co", [D, D])
    w_so = din("w_so", [D, D])
    w_o = din("w_o", [D, D])
    w_q = din("w_q", [D, 2 * D])
    dwT_d = din("dwT", [128, 8, 31])
    cvec_d = din("cvec", [128, 3, 8])
    sgg_d = din("sgg", [1, D])
    sgb_d = din("sgb", [1, D])
    wspT_d = din("wspT", [128, 8, 128])
    bsp_d = din("bsp", [1, D])
    subkT_d = din("subkT", [128, 16, 128])
    uv = din("uv", [16384, 2 * D])
    out = nc.dram_tensor("out", [S, D], F32, kind="ExternalOutput").ap()
    wsc = nc.dram_tensor("wsc", [11, 128, 8, D], BF16, kind="Internal").ap()
    uvb = nc.dram_tensor("uvb", [16384, 2 * D], BF16, kind="Internal").ap()
    dbg = None
    if debug == "og":
        dbg = nc.dram_tensor("dbg", [128, NT, D], BF16, kind="ExternalOutput").ap()

    es = ExitStack()
    with es:
        arena_t = es.enter_context(nc.sbuf_tensor("arena", [128, 53000], F32))
        A = Arena(arena_t[:, :])
        P = Prog(nc, es)
        psT = es.enter_context(nc.psum_tensor("psT", [128, 8, 128], BF16))
        banks = [es.enter_context(nc.psum_tensor("psb%d" % i, [128, 512], F32)) for i in range(7)]
        bank_rr = [0]

        def next_bank(n=7):
            i = bank_rr[0] % n
            bank_rr[0] += 1
            return i

        mod = A.alloc([6 * D], F32)
        gfb = A.alloc([D], F32)
        og = A.alloc([NT, D], BF16)
        subkT = A.alloc([16, 128], BF16)
        ident_f = A.alloc([128], F32)
        ident_b = A.alloc([128], BF16)
        ones_f = A.alloc([128], F32)
        ones_b = A.alloc([128], BF16)
        iota16 = A.alloc([16], F32)
        iota_i = A.alloc([16], I32)
        dwT = A.alloc([8, 31], F32)
        cvec = A.alloc([3, 8], F32)
        off_deadc = A.off
        sgg = A.alloc([D], F32)
        sgb = A.alloc([D], F32)
        bsp = A.alloc([8, 128], F32)
        WmT = A.alloc([8, 128], BF16)
        eps_t = A.alloc([2], F32)
        eps_ap = eps_t[:, 0:1]
        mark_global = A.off

        SH1, A1o, G1o, SH2, A2o, G2o = [i * D for i in range(6)]

        ct = A.alloc([8], F32)
        cact = A.alloc([8], F32)
        cbc = A.alloc([8, 128], BF16)
        gtmp = A.alloc([D], F32)
        wa = [A.alloc([8, 512], BF16) for _ in range(2)]
        stg = [A.alloc([8, D], BF16) for _ in range(2)]
        wsp_f = A.alloc([8, 128], F32)

        P.op("pool", lambda E: E.memset(ones_f, 1.0), writes=["ones_f"])
        P.op("pool", lambda E: E.tensor_copy(out=ones_b, in_=ones_f), reads=["ones_f"], writes=["ones_b"])
        P.op("pool", lambda E: E.affine_select(out=ident_f, in_=ones_f, pattern=[[-1, 128]],
                                               compare_op=ALU.is_equal, fill=0.0, base=0, channel_multiplier=1),
             reads=["ones_f"], writes=["ident_f"])
        P.op("pool", lambda E: E.tensor_copy(out=ident_b, in_=ident_f), reads=["ident_f"], writes=["ident_b"])
        P.op("pool", lambda E: E.iota(out=iota_i, pattern=[[1, 16]], base=0, channel_multiplier=0), writes=["iota_i"])
        P.op("pool", lambda E: E.tensor_copy(out=iota16, in_=iota_i), reads=["iota_i"], writes=["iota16"])

        P.dma("sp", lambda E: E.dma_start(out=ct, in_=c_t[:, :]), "k1", writes=["ct"])
        P.dma("sp", lambda E: E.dma_start(out=mod, in_=b_ada.partition_broadcast(128)[:, 0, :]), "k2", writes=["mod"])
        P.dma("sp", lambda E: E.dma_start(out=gfb, in_=gf.partition_broadcast(128)[:, 0, :]), "k3", writes=["gfb"])
        P.dma("pool", lambda E: E.dma_start(out=subkT, in_=subkT_d[:, :, :]), "k4", writes=["subkT"])
        P.op("act", lambda E: E.activation(out=cact, in_=ct, func=AF.Silu), reads=["ct"], writes=["cact"])
        P.op("dve", lambda E: E.tensor_copy(out=cbc, in_=cact.unsqueeze(2).to_broadcast([128, 8, 128])),
             reads=["cact"], writes=["cbc"])

        wada_v = w_ada.rearrange("(k p) n -> p k n", p=128)
        for nt in range(12):
            b = nt % 2
            P.dma("pool", lambda E, b=b, nt=nt: E.dma_start(out=wa[b], in_=wada_v[:, :, nt * 512:(nt + 1) * 512]),
                  "wa%d" % b, writes=["wa%d" % b])
            bk = next_bank()
            for k in range(8):
                P.op("pe", lambda E, b=b, k=k, bk=bk: E.matmul(banks[bk][:, :], lhsT=cbc[:, k, :], rhs=wa[b][:, k, :],
                                                               start=(k == 0), stop=(k == 7)),
                     reads=["cbc", "wa%d" % b], writes=["ps%d" % bk])
            P.op("dve", lambda E, nt=nt, bk=bk: E.tensor_tensor(out=mod[:, nt * 512:(nt + 1) * 512], in0=banks[bk][:, :],
                                                               in1=mod[:, nt * 512:(nt + 1) * 512], op=ALU.add),
                 reads=["ps%d" % bk], writes=["mod"])
        for (gd, off, sem) in ((g1, A1o, "k5"), (g2, A2o, "k6")):
            P.dma("sp", lambda E, gd=gd: E.dma_start(out=gtmp, in_=gd.partition_broadcast(128)[:, 0, :]), sem,
                  writes=["gtmp"])
            P.op("dve", lambda E, off=off: E.scalar_tensor_tensor(out=mod[:, off:off + D], in0=mod[:, off:off + D],
                                                                 scalar=1.0, in1=gtmp, op0=ALU.add, op1=ALU.mult),
                 reads=["gtmp"], writes=["mod"])

        P.dma("sp", lambda E: E.dma_start(out=dwT, in_=dwT_d[:, :, :]), "k7", writes=["dwT"])
        P.dma("sp", lambda E: E.dma_start(out=cvec, in_=cvec_d[:, :, :]), "k8", writes=["cvec"])
        P.dma("sp", lambda E: E.dma_start(out=sgg, in_=sgg_d.partition_broadcast(128)[:, 0, :]), "k9", writes=["sgg"])
        P.dma("sp", lambda E: E.dma_start(out=sgb, in_=sgb_d.partition_broadcast(128)[:, 0, :]), "k10", writes=["sgb"])
        P.dma("sp", lambda E: E.dma_start(out=bsp.rearrange("p a b -> p (a b)"),
                                          in_=bsp_d.partition_broadcast(128)[:, 0, :]), "k11", writes=["bsp"])
        P.dma("sp", lambda E: E.dma_start(out=wsp_f, in_=wspT_d[:, :, :]), "k12", writes=["wsp_f"])
        P.op("pool", lambda E: E.memset(wsp_f[64:128, :, 0:64], 0.0), writes=["wsp_f"])
        P.op("pool", lambda E: E.tensor_copy(out=WmT, in_=wsp_f), reads=["wsp_f"], writes=["WmT"])

        P.op("pool", lambda E: E.memset(eps_t, EPS), writes=["eps"])
        P.barrier(skip_prefix="c")
        A.off = mark_global

        secs = [w_in[:, i * D:(i + 1) * D] for i in range(6)] + [w_co, w_so, w_o, w_q[:, 0:D], w_q[:, D:2 * D]]
        for si, wsec in enumerate(secs):
            P.dma("pool", lambda E, si=si, wsec=wsec: E.dma_start(out=wsc[si], in_=wsec.rearrange("(k p) n -> p k n", p=128)),
                  "cw%d" % si, writes=["wsc%d" % si])
        NCV = 16
        for ci_ in range(NCV):
            r0 = ci_ * (16384 // NCV)
            P.dma("pool", lambda E, r0=r0: E.dma_start(
                out=uvb[r0:r0 + 16384 // NCV, :].rearrange("(a b) n -> a b n", a=8),
                in_=uv[r0:r0 + 16384 // NCV, :].rearrange("(a b) n -> a b n", a=8)),
                "cv%d" % ci_, writes=["uvb"])
        wring = [A.alloc([8, D], BF16) for _ in range(3)]
        xt = [A.alloc([D], F32) for _ in range(2)]
        nb = [A.alloc([D], BF16) for _ in range(2)]
        nT = A.alloc([8, TT], BF16)
        z = A.alloc([8, 30 + TT], BF16)
        acc = A.alloc([8, TT], F32)
        sq = A.alloc([8, TT], BF16)
        zs = A.alloc([8, TT], BF16)
        uT = A.alloc([8, TT], BF16)
        vtmp = A.alloc([D], F32)
        na = vtmp
        vn = [A.alloc([D], BF16) for _ in range(2)]
        sgA = A.alloc([8, TT], BF16)
        sgB = A.alloc([8, TT], BF16)
        gatedT = A.alloc([8, TT], BF16)
        m1 = A.alloc([8, TT], BF16)
        m2 = A.alloc([TT], F32)
        mergedT = A.alloc([8, TT], BF16)
        sgt = A.alloc([TT], F32)
        tmpg = A.alloc([TT], F32)
        mean_c = A.alloc([TT], F32)
        msq_c = A.alloc([TT], F32)
        rstd_c = A.alloc([TT], F32)
        small = A.alloc([64], F32)
        NDG = 12
        dgr = [A.alloc([128], BF16) for _ in range(NDG)]
        dgc = [0]
        print("phase1 arena words", A.off)

        P.op("dve", lambda E: E.memset(z[:, :, 0:30], 0.0), writes=["z%d" % c for c in range(8)])
        seq = [(tt, sec) for tt in range(NTT) for sec in range(9)]

        def issue_wload(n):
            if n >= len(seq):
                return
            _, sec = seq[n]
            sl = n % 3
            P.dma("sp", lambda E, sl=sl, sec=sec: E.dma_start(out=wring[sl], in_=wsc[sec]), "wr%d" % sl,
                  reads=["wsc%d" % sec], writes=["wr%d" % sl])

        issue_wload(0)
        issue_wload(1)

        def mm8(out_ap, lhs_fn, rhs_fn, reads, bk):
            for k in range(8):
                P.op("pe", lambda E, k=k: E.matmul(out_ap, lhsT=lhs_fn(k), rhs=rhs_fn(k), start=(k == 0), stop=(k == 7)),
                     reads=reads, writes=["ps%d" % bk])

        def rms_prep(src, sskey, pre, junk, junkkey):
            P.op("act", lambda E: E.activation(out=junk, in_=src, func=AF.Square, accum_out=small[:, pre:pre + 1]),
                 reads=[sskey], writes=[junkkey, "sm%d" % pre])
            P.op("act", lambda E: E.activation(out=small[:, pre + 1:pre + 2], in_=small[:, pre:pre + 1], func=AF.Sqrt,
                                               scale=1.0 / D, bias=eps_ap),
                 reads=["sm%d" % pre, "eps"], writes=["sm%d" % (pre + 1)])
            P.op("dve", lambda E: E.reciprocal(out=small[:, pre + 2:pre + 3], in_=small[:, pre + 1:pre + 2]),
                 reads=["sm%d" % (pre + 1)], writes=["sm%d" % (pre + 2)])


        for tt in range(NTT):
            n0 = tt * 9
            for s in range(2):
                if tt == 0:
                    P.dma("sp", lambda E, s=s: E.dma_start(out=xt[s], in_=x[s * 128:(s + 1) * 128, :]), "xt%d" % s,
                          writes=["xt%d" % s])
                pre = 4 * s
                rms_prep(xt[s], "xt%d" % s, pre, nb[s], "nb%d" % s)
                P.op("dve", lambda E, s=s, pre=pre: E.scalar_tensor_tensor(
                    out=na, in0=xt[s], scalar=small[:, pre + 2:pre + 3], in1=mod[:, A1o:A1o + D],
                    op0=ALU.mult, op1=ALU.mult), reads=["xt%d" % s, "sm%d" % (pre + 2), "mod"], writes=["vtmp"])
                P.op("dve", lambda E, s=s: E.tensor_tensor(out=nb[s], in0=na, in1=mod[:, SH1:SH1 + D], op=ALU.add),
                     reads=["vtmp", "mod"], writes=["nb%d" % s])
                if tt + 1 < NTT:
                    tokn = (tt + 1) * TT + s * 128
                    P.dma("sp", lambda E, s=s, tokn=tokn: E.dma_start(out=xt[s], in_=x[tokn:tokn + 128, :]), "xt%d" % s,
                          writes=["xt%d" % s])
                for k in range(8):
                    P.op("pe", lambda E, s=s, k=k: E.transpose(out=psT[:, k, :], in_=nb[s][:, k * 128:(k + 1) * 128],
                                                               identity=ident_b),
                         reads=["nb%d" % s, "ident_b"], writes=["psT"])
                P.op("act", lambda E, s=s: E.copy(out=nT[:, :, s * 128:(s + 1) * 128], in_=psT[:, :, :]),
                     reads=["psT"], writes=["nT"])

            issue_wload(n0 + 2)
            w0, w1 = wring[(n0) % 3], wring[(n0 + 1) % 3]
            k0, k1 = "wr%d" % (n0 % 3), "wr%d" % ((n0 + 1) % 3)
            issue_wload(n0 + 3) if False else None
            for c in range(8):
                bk = next_bank()
                mm8(banks[bk][:, 0:TT], lambda k, c=c: w0[:, k, c * 128:(c + 1) * 128], lambda k: nT[:, k, :],
                    [k0, "nT"], bk)
                mm8(banks[bk][:, TT:2 * TT], lambda k, c=c: w1[:, k, c * 128:(c + 1) * 128], lambda k: nT[:, k, :],
                    [k1, "nT"], bk)
                P.op("act", lambda E, bk=bk: E.activation(out=sgt, in_=banks[bk][:, TT:2 * TT], func=AF.Sigmoid),
                     reads=["ps%d" % bk], writes=["sgt"])
                P.op("dve", lambda E, bk=bk, c=c: E.tensor_tensor(out=z[:, c, 30:30 + TT], in0=banks[bk][:, 0:TT],
                                                                 in1=sgt, op=ALU.mult),
                     reads=["ps%d" % bk, "sgt"], writes=["z%d" % c])
            issue_wload(n0 + 3)
            issue_wload(n0 + 4)
            w2, k2 = wring[(n0 + 2) % 3], "wr%d" % ((n0 + 2) % 3)
            for c in range(8):
                bk = next_bank()
                mm8(banks[bk][:, 0:TT], lambda k, c=c: w2[:, k, c * 128:(c + 1) * 128], lambda k: nT[:, k, :],
                    [k2, "nT"], bk)
                P.op("act", lambda E, bk=bk, c=c: E.copy(out=uT[:, c, :], in_=banks[bk][:, 0:TT]),
                     reads=["ps%d" % bk], writes=["uT%d" % c])
            issue_wload(n0 + 5)
            w3, k3 = wring[(n0 + 3) % 3], "wr%d" % ((n0 + 3) % 3)
            for s in range(2):
                bks = [next_bank(), next_bank()]
                for nh in range(2):
                    mm8(banks[bks[nh]][:, :], lambda k, s=s: nT[:, k, s * 128:(s + 1) * 128],
                        lambda k, nh=nh: w3[:, k, nh * 512:(nh + 1) * 512], [k3, "nT"], bks[nh])
                    P.op("dve", lambda E, nh=nh, bks=bks: E.bn_stats(out=small[:, 16 + 6 * nh:22 + 6 * nh],
                                                                    in_=banks[bks[nh]][:, :]),
                         reads=["ps%d" % bks[nh]], writes=["bst%d" % nh])
                P.op("dve", lambda E: E.bn_aggr(out=small[:, 28:30], in_=small[:, 16:28]),
                     reads=["bst0", "bst1"], writes=["mv"])
                P.op("act", lambda E: E.activation(out=small[:, 30:31], in_=small[:, 29:30], func=AF.Sqrt, bias=eps_ap,
                                                   scale=1.0), reads=["mv", "eps"], writes=["vsd"])
                P.op("dve", lambda E: E.reciprocal(out=small[:, 31:32], in_=small[:, 30:31]), reads=["vsd"],
                     writes=["vrs"])
                for nh in range(2):
                    P.op("dve", lambda E, nh=nh, bks=bks: E.tensor_scalar(
                        out=vtmp[:, nh * 512:(nh + 1) * 512], in0=banks[bks[nh]][:, :], scalar1=small[:, 28:29],
                        scalar2=small[:, 31:32], op0=ALU.subtract, op1=ALU.mult),
                        reads=["ps%d" % bks[nh], "mv", "vrs"], writes=["vtmp"])
                P.op("dve", lambda E: E.tensor_tensor(out=vtmp, in0=vtmp, in1=sgg, op=ALU.mult),
                     reads=["sgg"], writes=["vtmp"])
                P.op("dve", lambda E, s=s: E.tensor_tensor(out=vn[s], in0=vtmp, in1=sgb, op=ALU.add),
                     reads=["vtmp", "sgb"], writes=["vn%d" % s])
            for g in range(8):
                bk = next_bank()
                for blk in range(2):
                    P.op("pe", lambda E, g=g, blk=blk, bk=bk: E.matmul(
                        banks[bk][:, blk * 128:(blk + 1) * 128], lhsT=vn[blk][:, g * 128:(g + 1) * 128], rhs=WmT[:, g, :],
                        start=True, stop=True), reads=["vn%d" % blk, "WmT"], writes=["ps%d" % bk])
                P.op("dve", lambda E, g=g, bk=bk: E.tensor_tensor(
                    out=tmpg.rearrange("p (a b) -> p a b", a=2), in0=banks[bk][:, 0:TT].rearrange("p (a b) -> p a b", a=2),
                    in1=bsp[:, g, :].unsqueeze(1).to_broadcast([128, 2, 128]), op=ALU.add),
                    reads=["ps%d" % bk, "bsp"], writes=["tmpg"])
                P.op("dve", lambda E, g=g: E.tensor_tensor(out=gatedT[:, g, :], in0=tmpg, in1=uT[:, g, :], op=ALU.mult),
                     reads=["tmpg", "uT%d" % g], writes=["gT%d" % g])
            for c in range(8):
                bk = next_bank()
                for w in range(31):
                    dsl = dgc[0] % NDG
                    dgc[0] += 1
                    if w % 3 == 2:
                        P.op("act", lambda E, c=c, w=w, dsl=dsl: E.activation(
                            out=dgr[dsl], in_=ident_b, func=AF.Copy, scale=dwT[:, c, w:w + 1]),
                            reads=["ident_b", "dwT"], writes=["dg%d" % dsl])
                    else:
                        P.op("dve", lambda E, c=c, w=w, dsl=dsl: E.tensor_scalar(
                            out=dgr[dsl], in0=ident_b, scalar1=dwT[:, c, w:w + 1], scalar2=None, op0=ALU.mult),
                            reads=["ident_b", "dwT"], writes=["dg%d" % dsl])
                    P.op("pe", lambda E, c=c, w=w, dsl=dsl, bk=bk: E.matmul(
                        banks[bk][:, 0:TT], lhsT=dgr[dsl], rhs=z[:, c, w:w + TT], start=(w == 0), stop=(w == 30)),
                        reads=["dg%d" % dsl, "z%d" % c], writes=["ps%d" % bk])
                P.op("act", lambda E, c=c, bk=bk: E.activation(out=acc[:, c, :], in_=banks[bk][:, 0:TT], func=AF.Identity,
                                                               bias=cvec[:, 0, c:c + 1]),
                     reads=["ps%d" % bk, "cvec"], writes=["acc%d" % c])
                P.op("act", lambda E, c=c: E.copy(out=z[:, c, 0:30], in_=z[:, c, TT:TT + 30]),
                     reads=["z%d" % c], writes=["z%d" % c])
                P.op("act", lambda E, c=c: E.activation(out=sq[:, c, :], in_=acc[:, c, :], func=AF.Square),
                     reads=["acc%d" % c], writes=["sq%d" % c])
            for (sidx, dst, nm) in ((4, sgA, "sgA"), (5, sgB, "sgB")):
                issue_wload(n0 + sidx + 2)
                wS, kS = wring[(n0 + sidx) % 3], "wr%d" % ((n0 + sidx) % 3)
                for c in range(8):
                    bk = next_bank()
                    mm8(banks[bk][:, 0:TT], lambda k, c=c, wS=wS: wS[:, k, c * 128:(c + 1) * 128], lambda k: nT[:, k, :],
                        [kS, "nT"], bk)
                    P.op("act", lambda E, bk=bk, c=c, dst=dst: E.activation(out=dst[:, c, :], in_=banks[bk][:, 0:TT],
                                                                           func=AF.Sigmoid),
                         reads=["ps%d" % bk], writes=["%s%d" % (nm, c)])
            bkS = next_bank()
            for c in range(8):
                P.op("pe", lambda E, c=c, bkS=bkS: E.matmul(banks[bkS][:, 0:TT], lhsT=ones_f, rhs=acc[:, c, :],
                                                   start=(c == 0), stop=(c == 7)),
                     reads=["ones_f", "acc%d" % c], writes=["ps%d" % bkS])
            for c in range(8):
                P.op("pe", lambda E, c=c, bkS=bkS: E.matmul(banks[bkS][:, TT:2 * TT], lhsT=ones_b, rhs=sq[:, c, :],
                                                   start=(c == 0), stop=(c == 7)),
                     reads=["ones_b", "sq%d" % c], writes=["ps%d" % bkS])
            P.op("dve", lambda E, bkS=bkS: E.tensor_scalar(out=mean_c, in0=banks[bkS][:, 0:TT], scalar1=1.0 / D, scalar2=None,
                                                  op0=ALU.mult), reads=["ps%d" % bkS], writes=["mean_c"])
            P.op("dve", lambda E: E.tensor_tensor(out=msq_c, in0=mean_c, in1=mean_c, op=ALU.mult),
                 reads=["mean_c"], writes=["msq_c"])
            P.op("dve", lambda E, bkS=bkS: E.scalar_tensor_tensor(out=msq_c, in0=banks[bkS][:, TT:2 * TT], scalar=1.0 / D,
                                                         in1=msq_c, op0=ALU.mult, op1=ALU.subtract),
                 reads=["ps%d" % bkS, "msq_c"], writes=["msq_c"])
            P.op("act", lambda E: E.activation(out=rstd_c, in_=msq_c, func=AF.Sqrt, bias=eps_ap, scale=1.0),
                 reads=["msq_c", "eps"], writes=["rstd_c"])
            P.op("dve", lambda E: E.reciprocal(out=rstd_c, in_=rstd_c), reads=["rstd_c"], writes=["rstd_c"])
            for c in range(8):
                P.op("dve", lambda E, c=c: E.tensor_tensor(out=acc[:, c, :], in0=acc[:, c, :], in1=mean_c,
                                                          op=ALU.subtract),
                     reads=["mean_c"], writes=["acc%d" % c])
                P.op("dve", lambda E, c=c: E.tensor_tensor(out=acc[:, c, :], in0=acc[:, c, :], in1=rstd_c, op=ALU.mult),
                     reads=["rstd_c"], writes=["acc%d" % c])
                P.op("act", lambda E, c=c: E.activation(out=zs[:, c, :], in_=acc[:, c, :], func=AF.Silu,
                                                        scale=cvec[:, 1, c:c + 1], bias=cvec[:, 2, c:c + 1]),
                     reads=["acc%d" % c, "cvec"], writes=["zs%d" % c])
            issue_wload(n0 + 8)
            w6, k6 = wring[(n0 + 6) % 3], "wr%d" % ((n0 + 6) % 3)
            for dc in range(8):
                bk = next_bank()
                for c in range(8):
                    P.op("pe", lambda E, c=c, dc=dc, bk=bk: E.matmul(banks[bk][:, 0:TT], lhsT=w6[:, c, dc * 128:(dc + 1) * 128],
                                                                    rhs=zs[:, c, :], start=(c == 0), stop=(c == 7)),
                         reads=[k6, "zs%d" % c], writes=["ps%d" % bk])
                P.op("dve", lambda E, dc=dc, bk=bk: E.tensor_tensor(out=m1[:, dc, :], in0=banks[bk][:, 0:TT],
                                                                   in1=sgA[:, dc, :], op=ALU.mult),
                     reads=["ps%d" % bk, "sgA%d" % dc], writes=["m1_%d" % dc])
            issue_wload(n0 + 9)
            w7, k7 = wring[(n0 + 7) % 3], "wr%d" % ((n0 + 7) % 3)
            for dc in range(8):
                bk = next_bank()
                for c in range(8):
                    P.op("pe", lambda E, c=c, dc=dc, bk=bk: E.matmul(banks[bk][:, 0:TT], lhsT=w7[:, c, dc * 128:(dc + 1) * 128],
                                                                    rhs=gatedT[:, c, :], start=(c == 0), stop=(c == 7)),
                         reads=[k7, "gT%d" % c], writes=["ps%d" % bk])
                P.op("dve", lambda E, dc=dc, bk=bk: E.tensor_tensor(out=m2, in0=banks[bk][:, 0:TT], in1=sgB[:, dc, :],
                                                                   op=ALU.mult),
                     reads=["ps%d" % bk, "sgB%d" % dc], writes=["m2"])
                P.op("dve", lambda E, dc=dc: E.tensor_tensor(out=mergedT[:, dc, :], in0=m1[:, dc, :], in1=m2, op=ALU.add),
                     reads=["m1_%d" % dc, "m2"], writes=["mg%d" % dc])
            issue_wload(n0 + 10)
            w8, k8 = wring[(n0 + 8) % 3], "wr%d" % ((n0 + 8) % 3)
            for s in range(2):
                tile = tt * 2 + s
                for nh in range(2):
                    bk = next_bank()
                    for dc in range(8):
                        P.op("pe", lambda E, s=s, nh=nh, dc=dc, bk=bk: E.matmul(
                            banks[bk][:, :], lhsT=mergedT[:, dc, s * 128:(s + 1) * 128],
                            rhs=w8[:, dc, nh * 512:(nh + 1) * 512], start=(dc == 0), stop=(dc == 7)),
                            reads=[k8, "mg%d" % dc], writes=["ps%d" % bk])
                    P.op("dve", lambda E, tile=tile, nh=nh, bk=bk: E.tensor_tensor(
                        out=og[:, tile, nh * 512:(nh + 1) * 512], in0=banks[bk][:, :],
                        in1=mod[:, G1o + nh * 512:G1o + (nh + 1) * 512], op=ALU.mult),
                        reads=["ps%d" % bk, "mod"], writes=["og%d" % tile])

        if debug == "og":
            P.dma("sp", lambda E: E.dma_start(out=dbg[:, 0:2 * NTT, :], in_=og[:, 0:2 * NTT, :]), "dbg",
                  reads=["og%d" % t for t in range(2 * NTT)])
            P.finish(["dbg"])
            P.emit()
            return nc

        P.barrier(skip_prefix="c")
        A.off = mark_global

        IOA = bass.IndirectOffsetOnAxis
        wq = A.alloc([8, 2 * D], BF16)
        def _bf(ap_f32, nslots):
            return ap_f32.bitcast(BF16).rearrange("p (a b) -> p a b", a=nslots)
        arena_ap = arena_t[:, :]
        ringb = []
        for _ in range(3):
            t_ = A.alloc([BATCH, 2 * D], BF16)
            ringb.append(([t_[:, jj, :] for jj in range(BATCH)], t_[:, 2:4, :]))
        deadc = arena_ap[:, off_deadc:off_deadc + 3072]
        pair5 = _bf(deadc[:, 0:2048], 2)
        s5a = _bf(deadc[:, 2048:3072], 1)
        s5b = _bf(mod[:, 1024:2048], 1)
        ringb.append(([s5a[:, 0, :], s5b[:, 0, :], pair5[:, 0, :], pair5[:, 1, :]], pair5))
        NRB = len(ringb)
        prod = _bf(mod[:, 0:1024], 2)
        junkA = mod[:, 2048:2560].bitcast(BF16)
        junkD = A.alloc([D], BF16)
        n2T = mod[:, 2560:3072].bitcast(BF16).rearrange("p (a b) -> p a b", a=8)
        h1 = [A.alloc([D], F32) for _ in range(3)]
        n2b = [A.alloc([D], BF16) for _ in range(2)]
        eidx = [A.alloc([8, 16], I32) for _ in range(2)]
        gate = [A.alloc([8, 16], F32) for _ in range(2)]
        n2 = A.alloc([D], F32)
        h2 = [n2, n2]
        vals = A.alloc([8, 2, 16], F32)
        idxs = A.alloc([8, 2, 16], U32)
        tmpm = [A.alloc([128], F32) for _ in range(2)]
        cand = A.alloc([8, 16, 16], F32)
        qT = cand.rearrange("p a b c -> p (a b c)")[:, 0:1024].bitcast(BF16).rearrange("p (a b) -> p a b", a=16)
        tmpc = [A.alloc([256], F32) for _ in range(2)]
        st = A.alloc([8, 16], F32)
        ci = A.alloc([8, 16], U32)
        ar = A.alloc([8, 16], U32)
        br = A.alloc([8, 16], U32)
        eq = cand
        i1sel = A.alloc([8, 16], F32)
        i2sel = A.alloc([8, 16], F32)
        ex = A.alloc([8, 16], F32)
        ssum = A.alloc([8], F32)
        av = A.alloc([NSLOT], F32)
        gsg = [A.alloc([BATCH], F32) for _ in range(4)]
        hg = A.alloc([NSLOT], F32)
        NDIAG = 2 * BATCH
        diag = [A.alloc([128], BF16) for _ in range(NDIAG)]
        sm2 = A.alloc([64], F32)
        print("phase2 arena words", A.off)


        for half in range(2):
            P.dma("sp", lambda E, half=half: E.dma_start(out=wq[:, :, half * D:(half + 1) * D], in_=wsc[9 + half]),
                  "wq%d" % half, reads=["wsc%d" % (9 + half)], writes=["wq"])

        def rms2(src, sskey, pre, jk, jkkey):
            P.op("act", lambda E: E.activation(out=jk, in_=src, func=AF.Square, accum_out=sm2[:, pre:pre + 1]),
                 reads=[sskey], writes=[jkkey, "s2_%d" % pre])
            P.op("act", lambda E: E.activation(out=sm2[:, pre + 1:pre + 2], in_=sm2[:, pre:pre + 1], func=AF.Sqrt,
                                               scale=1.0 / D, bias=eps_ap),
                 reads=["s2_%d" % pre, "eps"], writes=["s2_%d" % (pre + 1)])
            P.op("dve", lambda E: E.reciprocal(out=sm2[:, pre + 2:pre + 3], in_=sm2[:, pre + 1:pre + 2]),
                 reads=["s2_%d" % (pre + 1)], writes=["s2_%d" % (pre + 2)])

        def sc_ap(hp):
            return banks[hp // 4][:, (hp % 4) * 128:(hp % 4 + 1) * 128]

        def top16_multi(chains):
            for stage in range(5):
                for (src_fn, srckeys, vout_fn, iout_fn, vkey, ikey, tmpbuf, tmpkey) in chains:
                    if stage == 0:
                        P.op("dve", lambda E, src_fn=src_fn, vout_fn=vout_fn: E.max(out=vout_fn(0), in_=src_fn()),
                             reads=srckeys, writes=[vkey + "a"])
                    elif stage == 1:
                        P.op("dve", lambda E, src_fn=src_fn, vout_fn=vout_fn, iout_fn=iout_fn: E.max_index(
                            out=iout_fn(0), in_max=vout_fn(0), in_values=src_fn()),
                            reads=srckeys + [vkey + "a"], writes=[ikey + "a"])
                    elif stage == 2:
                        P.op("dve", lambda E, src_fn=src_fn, vout_fn=vout_fn, tmpbuf=tmpbuf: E.match_replace(
                            out=tmpbuf, in_to_replace=vout_fn(0), in_values=src_fn(), imm_value=-1e30),
                            reads=srckeys + [vkey + "a"], writes=[tmpkey])
                    elif stage == 3:
                        P.op("dve", lambda E, vout_fn=vout_fn, tmpbuf=tmpbuf: E.max(out=vout_fn(1), in_=tmpbuf),
                             reads=[tmpkey], writes=[vkey + "b"])
                    else:
                        P.op("dve", lambda E, vout_fn=vout_fn, iout_fn=iout_fn, tmpbuf=tmpbuf: E.max_index(
                            out=iout_fn(1), in_max=vout_fn(1), in_values=tmpbuf),
                            reads=[tmpkey, vkey + "b"], writes=[ikey + "b"])

        def prep_load(i):
            b3 = i % 3
            P.dma("sp", lambda E: E.dma_start(out=h1[b3], in_=x[i * 128:(i + 1) * 128, :]), "h1_%d" % b3,
                  writes=["h1_%d" % b3])

        def prep(i):
            b = i % 2
            b3 = i % 3
            pre = 4 * b
            P.op("dve", lambda E: E.tensor_tensor(out=h1[b3], in0=h1[b3], in1=og[:, i, :], op=ALU.add),
                 reads=["og%d" % i], writes=["h1_%d" % b3])
            yield "x"
            P.op("act", lambda E: E.activation(out=junkA, in_=h1[b3], func=AF.Square, accum_out=sm2[:, pre:pre + 1]),
                 reads=["h1_%d" % b3], writes=["junkA", "s2_%d" % pre])
            P.op("act", lambda E: E.activation(out=sm2[:, pre + 1:pre + 2], in_=sm2[:, pre:pre + 1], func=AF.Sqrt,
                                               scale=1.0 / D, bias=eps_ap),
                 reads=["s2_%d" % pre, "eps"], writes=["s2_%d" % (pre + 1)])
            yield "x"
            P.op("dve", lambda E: E.reciprocal(out=sm2[:, pre + 2:pre + 3], in_=sm2[:, pre + 1:pre + 2]),
                 reads=["s2_%d" % (pre + 1)], writes=["s2_%d" % (pre + 2)])
            P.op("dve", lambda E: E.scalar_tensor_tensor(out=n2, in0=h1[b3], scalar=sm2[:, pre + 2:pre + 3],
                                                         in1=mod[:, A2o:A2o + D], op0=ALU.mult, op1=ALU.mult),
                 reads=["h1_%d" % b3, "s2_%d" % (pre + 2), "mod"], writes=["n2"])
            P.op("dve", lambda E: E.tensor_tensor(out=n2b[b], in0=n2, in1=mod[:, SH2:SH2 + D], op=ALU.add),
                 reads=["n2", "mod"], writes=["n2b%d" % b])
            yield "x"
            for k in range(8):
                P.op("pe", lambda E, k=k: E.transpose(out=psT[:, k, :], in_=n2b[b][:, k * 128:(k + 1) * 128],
                                                      identity=ident_b),
                     reads=["n2b%d" % b, "ident_b"], writes=["psT"])
            yield "x"
            P.op("act", lambda E: E.copy(out=n2T, in_=psT[:, :, :]), reads=["psT"], writes=["n2T"])
            yield "x"
            for gq in range(4):
                for jj in range(4):
                    hp = gq * 4 + jj
                    mm8(banks[6][:, jj * 128:(jj + 1) * 128], lambda k, hp=hp: wq[:, k, hp * 128:(hp + 1) * 128],
                        lambda k: n2T[:, k, :], ["wq", "n2T"], 6)
                yield "x"
                P.op("act", lambda E, gq=gq: E.copy(out=qT[:, gq * 4:(gq + 1) * 4, :],
                                                    in_=banks[6][:, :].rearrange("p (a b) -> p a b", a=4)),
                     reads=["ps6"], writes=["cand"])
                yield "x"
            for hp in range(16):
                P.op("pe", lambda E, hp=hp: E.matmul(sc_ap(hp), lhsT=qT[:, hp, :], rhs=subkT[:, hp, :], start=True,
                                                     stop=True),
                     reads=["cand", "subkT"], writes=["ps%d" % (hp // 4)])
            yield "x"
            for hp0 in range(0, 16, 2):
                chains = []
                for hp in (hp0, hp0 + 1):
                    h, p = hp // 2, hp % 2
                    chains.append((lambda hp=hp: sc_ap(hp), ["ps%d" % (hp // 4)],
                                   lambda r, h=h, p=p: vals[:, h, p, 8 * r:8 * r + 8],
                                   lambda r, h=h, p=p: idxs[:, h, p, 8 * r:8 * r + 8],
                                   "vals%d" % hp, "idxs%d" % hp, tmpm[hp % 2], "tmpm%d" % (hp % 2)))
                top16_multi(chains)
                yield "d"
            P.op("dve", lambda E: E.tensor_tensor(
                out=cand, in0=vals[:, :, 0, :].unsqueeze(3).to_broadcast([128, 8, 16, 16]),
                in1=vals[:, :, 1, :].unsqueeze(2).to_broadcast([128, 8, 16, 16]), op=ALU.add),
                reads=["vals%d%s" % (hp, ab) for hp in range(16) for ab in "ab"], writes=["cand"])
            for h0 in range(0, 8, 2):
                chains = []
                for h in (h0, h0 + 1):
                    chains.append((lambda h=h: cand[:, h, :, :].rearrange("p a b -> p (a b)"), ["cand"],
                                   lambda r, h=h: st[:, h, 8 * r:8 * r + 8], lambda r, h=h: ci[:, h, 8 * r:8 * r + 8],
                                   "st%d" % h, "ci%d" % h, tmpc[h % 2], "tmpc%d" % (h % 2)))
                top16_multi(chains)
                yield "d"
            P.op("dve", lambda E: E.tensor_single_scalar(out=ar, in_=ci, scalar=4, op=ALU.logical_shift_right),
                 reads=["ci%d%s" % (h, ab) for h in range(8) for ab in "ab"], writes=["ar"])
            P.op("dve", lambda E: E.tensor_single_scalar(out=br, in_=ci, scalar=15, op=ALU.bitwise_and),
                 reads=["ci%d%s" % (h, ab) for h in range(8) for ab in "ab"], writes=["br"])
            for (sel, src, pp, key) in ((i1sel, ar, 0, "i1sel"), (i2sel, br, 1, "i2sel")):
                P.op("dve", lambda E, src=src: E.tensor_tensor(
                    out=eq, in0=src.unsqueeze(3).to_broadcast([128, 8, 16, 16]),
                    in1=iota16.unsqueeze(1).unsqueeze(1).to_broadcast([128, 8, 16, 16]), op=ALU.is_equal),
                    reads=["ar", "br", "iota16"], writes=["cand"])
                P.op("dve", lambda E, pp=pp: E.tensor_tensor(
                    out=eq, in0=eq, in1=idxs[:, :, pp, :].unsqueeze(2).to_broadcast([128, 8, 16, 16]), op=ALU.mult),
                    reads=["idxs%d%s" % (hp, ab) for hp in range(16) for ab in "ab"], writes=["cand"])
                P.op("dve", lambda E, sel=sel: E.tensor_reduce(out=sel, in_=eq, axis=AX.X, op=ALU.add),
                     reads=["cand"], writes=[key])
                yield "d"
            P.op("dve", lambda E: E.scalar_tensor_tensor(out=eidx[b], in0=i1sel, scalar=128.0, in1=i2sel,
                                                         op0=ALU.mult, op1=ALU.add),
                 reads=["i1sel", "i2sel"], writes=["eidx%d" % b])
            P.op("dve", lambda E: E.tensor_tensor(out=ex, in0=st, in1=st[:, :, 0:1].to_broadcast([128, 8, 16]),
                                                  op=ALU.subtract), reads=["st%d%s" % (h, ab) for h in range(8) for ab in "ab"], writes=["ex"])
            yield "x"
            P.op("act", lambda E: E.activation(out=ex, in_=ex, func=AF.Exp), reads=["ex"], writes=["ex"])
            yield "x"
            P.op("dve", lambda E: E.tensor_reduce(out=ssum, in_=ex, axis=AX.X, op=ALU.add), reads=["ex"],
                 writes=["ssum"])
            P.op("dve", lambda E: E.reciprocal(out=ssum, in_=ssum), reads=["ssum"], writes=["ssum"])
            P.op("dve", lambda E: E.tensor_tensor(out=gate[b], in0=ex, in1=ssum.unsqueeze(2).to_broadcast([128, 8, 16]),
                                                  op=ALU.mult), reads=["ex", "ssum"], writes=["gate%d" % b])
            yield "x"

        gcount = [0]
        breg = [None]
        P.stream["pool"].append(("raw", lambda E: breg.__setitem__(0, E.to_reg(16383))))

        def compute(i, mid_hook):
            b = i % 2
            eflat = eidx[b].rearrange("p a b -> p (a b)")
            gflat = gate[b].rearrange("p a b -> p (a b)")
            NSTT = 2

            def gathers(j0):
                rb = gcount[0] % NRB
                gcount[0] += 1
                rslots, rpair = ringb[rb]
                for jj in range(BATCH):
                    j = j0 + jj
                    P.dma("pool", lambda E, j=j, jj=jj, rslots=rslots: E.indirect_dma_start(
                        out=rslots[jj], out_offset=None, in_=uvb[:, :], in_offset=IOA(ap=eflat[:, j:j + 1], axis=0),
                        bounds_check=breg[0], oob_is_err=False),
                        "g%d_%d" % (rb, jj), reads=["eidx%d" % b, "uvb"], writes=["ring%d_%d" % (rb, jj)])
                return (j0, rb, rslots, rpair)

            def products(ctx):
                j0, rb, rslots, rpair = ctx
                rkeys = ["ring%d_%d" % (rb, jj) for jj in range(BATCH)]
                for jj in range(NSTT):
                    j = j0 + jj
                    P.op("dve", lambda E, rslots=rslots, j=j, jj=jj: E.scalar_tensor_tensor(
                        out=junkD, in0=rslots[jj][:, 0:D], scalar=1.0, in1=n2b[b], op0=ALU.mult, op1=ALU.mult,
                        accum_out=av[:, j:j + 1]),
                        reads=[rkeys[jj], "n2b%d" % b], writes=["junkD", "av%d" % j])
                P.op("dve", lambda E, rpair=rpair: E.tensor_tensor(
                    out=prod, in0=rpair[:, :, 0:D],
                    in1=n2b[b].unsqueeze(1).to_broadcast([128, BATCH - NSTT, D]), op=ALU.mult),
                    reads=rkeys[NSTT:] + ["n2b%d" % b], writes=["prod"])

            def reduces(ctx):
                j0 = ctx[0]
                for jj in range(NSTT, BATCH):
                    j = j0 + jj
                    P.op("act", lambda E, j=j, jj=jj: E.activation(out=junkA, in_=prod[:, jj - NSTT, :], func=AF.Copy,
                                                                   accum_out=av[:, j:j + 1]),
                         reads=["prod"], writes=["junkA", "av%d" % j])

            def tail_gelu(ctx):
                j0 = ctx[0]
                akeys = ["av%d" % j for j in range(j0, j0 + BATCH)]
                gs = gsg[(j0 // BATCH) % 4]
                gk = "gsg%d" % ((j0 // BATCH) % 4)
                P.op("act", lambda E: E.activation(out=gs, in_=av[:, j0:j0 + BATCH], func=AF.Gelu_apprx_tanh),
                     reads=akeys, writes=[gk])

            def tail_rest(ctx):
                j0, rb, rslots, rpair = ctx
                gs = gsg[(j0 // BATCH) % 4]
                gk = "gsg%d" % ((j0 // BATCH) % 4)
                P.op("dve", lambda E: E.tensor_tensor(out=hg[:, j0:j0 + BATCH], in0=gs, in1=gflat[:, j0:j0 + BATCH],
                                                      op=ALU.mult),
                     reads=[gk, "gate%d" % b], writes=["hg%d" % (j0 // BATCH)])
                for jj, j in enumerate(range(j0, j0 + BATCH)):
                    ds = j % NDIAG
                    P.op("act", lambda E, j=j, ds=ds: E.activation(out=diag[ds], in_=ident_b, func=AF.Copy,
                                                                   scale=hg[:, j:j + 1]),
                         reads=["ident_b", "hg%d" % (j0 // BATCH)], writes=["diag%d" % ds])
                    for half in range(2):
                        P.op("pe", lambda E, j=j, ds=ds, jj=jj, half=half, rslots=rslots: E.matmul(
                            banks[4 + half][:, :], lhsT=diag[ds], rhs=rslots[jj][:, D + half * 512:D + (half + 1) * 512],
                            start=(j == 0), stop=(j == NSLOT - 1)),
                            reads=["diag%d" % ds, "ring%d_%d" % (rb, jj)], writes=["ps%d" % (4 + half)])

            ctxs = []
            nb_ = NSLOT // BATCH
            for kb in range(nb_ + 1):
                if mid_hook is not None and kb >= 1:
                    tag = next(mid_hook, None)
                    if tag == "d" and kb >= 28:
                        next(mid_hook, None)
                if kb < nb_:
                    ctxs.append(gathers(kb * BATCH))
                if kb >= 1:
                    tail_gelu(ctxs[kb - 1])
                if kb < nb_:
                    products(ctxs[kb])
                    reduces(ctxs[kb])
                if kb >= 1:
                    tail_rest(ctxs[kb - 1])
            if mid_hook is not None:
                for _ in mid_hook:
                    pass
            for half in range(2):
                P.op("dve", lambda E, half=half: E.tensor_tensor(
                    out=h2[b][:, half * 512:(half + 1) * 512], in0=banks[4 + half][:, :],
                    in1=mod[:, G2o + half * 512:G2o + (half + 1) * 512], op=ALU.mult),
                    reads=["ps%d" % (4 + half), "mod"], writes=["n2"])
            P.op("dve", lambda E: E.tensor_tensor(out=h2[b], in0=h2[b], in1=h1[i % 3], op=ALU.add),
                 reads=["h1_%d" % (i % 3)], writes=["n2"])
            pre = 16 + 4 * b
            rms2(h2[b], "n2", pre, junkA, "junkA")
            P.op("dve", lambda E: E.scalar_tensor_tensor(out=h2[b], in0=h2[b], scalar=sm2[:, pre + 2:pre + 3], in1=gfb,
                                                         op0=ALU.mult, op1=ALU.mult),
                 reads=["s2_%d" % (pre + 2), "gfb"], writes=["n2"])
            P.dma("sp", lambda E: E.dma_start(out=out[i * 128:(i + 1) * 128, :], in_=h2[b]), "o%d" % b,
                  reads=["n2"])

        NT2 = NT if debug is None else int(debug[2:])
        prep_load(0)
        for _ in prep(0):
            pass
        if NT2 > 1:
            prep_load(1)
        for i in range(NT2):
            if i + 2 < NT2:
                prep_load(i + 2)
            hook = prep(i + 1) if i + 1 < NT2 else None
            compute(i, hook)
        P.finish([k for k in ("o0", "o1") if k in P.dsem])
        P.emit()
    return nc


def _prep_inputs(inputs):
    f = lambda a: np.ascontiguousarray(np.asarray(a, dtype=np.float32))
    x = f(inputs["x"])
    c = f(inputs["c"])
    shared = {
        "w_ada": f(inputs["w_ada"][0]),
        "b_ada": f(inputs["b_ada"][0]).reshape(1, -1),
        "g1": f(inputs["g_norm1"][0]).reshape(1, -1),
        "g2": f(inputs["g_norm2"][0]).reshape(1, -1),
        "gf": f(inputs["g_final"]).reshape(1, -1),
        "w_in": f(inputs["w_in"][0]),
        "w_co": f(inputs["w_conv_out"][0]),
        "w_so": f(inputs["w_sgu_out"][0]),
        "w_o": f(inputs["w_out"][0]),
        "w_q": f(inputs["w_query"][0]),
        "dwT": f(np.asarray(inputs["conv_dw_w"][0]).T.reshape(8, 128, 31).transpose(1, 0, 2)),
        "cvec": f(np.stack([np.asarray(inputs[k][0]).reshape(8, 128).T for k in
                            ("conv_dw_b", "conv_ln_g", "conv_ln_b")], axis=1)),
        "sgg": f(inputs["sgu_ln_g"][0]).reshape(1, -1),
        "sgb": f(inputs["sgu_ln_b"][0]).reshape(1, -1),
        "wspT": f(np.transpose(np.asarray(inputs["w_spatial"][0]), (2, 0, 1))),
        "bsp": f(inputs["b_spatial"][0]).reshape(1, -1),
        "subkT": f(np.transpose(np.asarray(inputs["sub_keys"][0]).reshape(16, 128, 128), (2, 0, 1))),
        "uv": f(np.concatenate([np.asarray(inputs["expert_u"][0]), np.asarray(inputs["expert_v"][0])], axis=1)),
    }
    in_maps = []
    for b in range(8):
        m = dict(shared)
        m["x"] = np.ascontiguousarray(x[b])
        m["c_t"] = np.ascontiguousarray(c[b].reshape(8, 128).T)
        in_maps.append(m)
    return in_maps


def kernel(**inputs):
    in_maps = _prep_inputs(inputs)
    nc = build_nc()
    res = run_bass_kernel_spmd(nc, in_maps, core_ids=list(range(8)))
    return np.stack([np.asarray(r["out"], dtype=np.float32) for r in res.results], axis=0)
```

```python
import math
from contextlib import ExitStack

import numpy as np
import concourse.bass as bass
import concourse.mybir as mybir
from concourse.bass_utils import run_bass_kernel_spmd

F32 = mybir.dt.float32
BF16 = mybir.dt.bfloat16
U32 = mybir.dt.uint32
I32 = mybir.dt.int32
AF = mybir.ActivationFunctionType
ALU = mybir.AluOpType
AX = mybir.AxisListType

D = 1024
S = 2048
NT = S // 128
TT = 256
NTT = S // TT
EPS = 1e-6
NSLOT = 128
RING = 16
BATCH = 4
GELU_C = 2.0 * math.sqrt(2.0 / math.pi)

_DSIZE = {F32: 4, BF16: 2, U32: 4, I32: 4}


class Arena:
    def __init__(self, base):
        self.base = base
        self.off = 0
        self.cap = base.shape[1]

    def alloc(self, free_shape, dtype):
        n = 1
        for d in free_shape:
            n *= d
        words = (n * _DSIZE[dtype] + 3) // 4
        words = (words + 1) // 2 * 2
        assert self.off + words <= self.cap, ("arena overflow", self.off, words, self.cap)
        ap = self.base[:, self.off:self.off + words]
        if dtype != F32:
            ap = ap.bitcast(dtype)
        ap = ap[:, 0:n]
        if len(free_shape) == 2:
            ap = ap.rearrange("p (a b) -> p a b", a=free_shape[0])
        elif len(free_shape) == 3:
            ap = ap.rearrange("p (a b c) -> p a b c", a=free_shape[0], b=free_shape[1])
        self.off += words
        return ap


class Prog:
    def __init__(self, nc, es):
        self.nc = nc
        self.es = es
        self.names = {"pe": "tensor", "act": "scalar", "dve": "vector", "pool": "gpsimd", "sp": "sync"}
        self.stream = {e: [] for e in self.names}
        self.esem = {e: es.enter_context(nc.semaphore("se_" + e)) for e in self.names}
        self.ecnt = {e: 0 for e in self.names}
        self.dsem = {}
        self.dcnt = {}
        self.lastw = {}
        self.readers = {}
        self.waited = {e: {} for e in self.names}

    def _need(self, e, tok, same_ok):
        semkey, val, src = tok
        if same_ok and src == e and e == "pe":
            return
        if semkey[0] == "d":
            val = self.dcnt[semkey[1]]
        if self.waited[e].get(semkey, 0) >= val:
            return
        self.waited[e][semkey] = val
        self.stream[e].append(("w", semkey, val))

    def _deps(self, e, reads, writes):
        for k in reads:
            t = self.lastw.get(k)
            if t is not None:
                self._need(e, t, same_ok=False)
        for k in writes:
            t = self.lastw.get(k)
            if t is not None:
                self._need(e, t, same_ok=True)
            for sk, (v, src) in self.readers.get(k, {}).items():
                self._need(e, (sk, v, src), same_ok=True)

    def _record(self, tok, reads, writes):
        for k in writes:
            self.lastw[k] = tok
            self.readers[k] = {}
        for k in reads:
            if k in writes:
                continue
            self.readers.setdefault(k, {})[tok[0]] = (tok[1], tok[2])

    def op(self, e, fn, reads=(), writes=()):
        self._deps(e, reads, writes)
        self.ecnt[e] += 1
        tok = (("e", e), self.ecnt[e], e)
        self.stream[e].append(("op", fn))
        self._record(tok, reads, writes)

    def dma(self, q, fn, sem, reads=(), writes=()):
        if sem not in self.dsem:
            self.dsem[sem] = self.es.enter_context(self.nc.semaphore("sd_" + sem))
            self.dcnt[sem] = 0
        self._deps(q, reads, writes)
        if self.dcnt[sem] > 0:
            self._need(q, (("d", sem), self.dcnt[sem], None), same_ok=False)
        self.dcnt[sem] += 16
        tok = (("d", sem), self.dcnt[sem], None)
        self.stream[q].append(("dma", fn, sem))
        self._record(tok, reads, writes)

    def barrier(self, skip_prefix=None):
        for e in self.names:
            for e2 in self.names:
                if e2 != e and self.ecnt[e2] > 0:
                    self._need(e, (("e", e2), self.ecnt[e2], e2), same_ok=False)
            for sname in self.dsem:
                if skip_prefix is not None and sname.startswith(skip_prefix):
                    continue
                if self.dcnt[sname] > 0:
                    self._need(e, (("d", sname), self.dcnt[sname], None), same_ok=False)

    def finish(self, sems):
        for sname in sems:
            self._need("sp", (("d", sname), self.dcnt[sname], None), same_ok=False)

    def _semobj(self, semkey):
        return self.esem[semkey[1]] if semkey[0] == "e" else self.dsem[semkey[1]]

    def emit(self):
        with self.nc.Block() as block:
            for e, nm in self.names.items():
                def body(E, e=e):
                    for item in self.stream[e]:
                        if item[0] == "w":
                            E.wait_ge(self._semobj(item[1]), item[2])
                        elif item[0] == "raw":
                            item[1](E)
                        elif item[0] == "op":
                            item[1](E).then_inc(self.esem[e], 1)
                        else:
                            item[1](E).then_inc(self.dsem[item[2]], 16)
                getattr(block, nm)(body)


def build_nc(debug=None):
    nc = bass.Bass("TRN2", target_bir_lowering=False)

    def din(name, shape, dt=F32):
        return nc.dram_tensor(name, list(shape), dt, kind="ExternalInput").ap()

    x = din("x", [S, D])
    c_t = din("c_t", [128, 8])
    w_ada = din("w_ada", [D, 6 * D])
    b_ada = din("b_ada", [1, 6 * D])
    g1 = din("g1", [1, D])
    g2 = din("g2", [1, D])
    gf = din("gf", [1, D])
    w_in = din("w_in", [D, 6 * D])
    w_co = din("w_co", [D, D])
    w_so = din("w_so", [D, D])
    w_o = din("w_o", [D, D])
    w_q = din("w_q", [D, 2 * D])
    dwT_d = din("dwT", [128, 8, 31])
    cvec_d = din("cvec", [128, 3, 8])
    sgg_d = din("sgg", [1, D])
    sgb_d = din("sgb", [1, D])
    wspT_d = din("wspT", [128, 8, 128])
    bsp_d = din("bsp", [1, D])
    subkT_d = din("subkT", [128, 16, 128])
    uv = din("uv", [16384, 2 * D])
    out = nc.dram_tensor("out", [S, D], F32, kind="ExternalOutput").ap()
    wsc = nc.dram_tensor("wsc", [11, 128, 8, D], BF16, kind="Internal").ap()
    uvb = nc.dram_tensor("uvb", [16384, 2 * D], BF16, kind="Internal").ap()
    dbg = None
    if debug == "og":
        dbg = nc.dram_tensor("dbg", [128, NT, D], BF16, kind="ExternalOutput").ap()

    es = ExitStack()
    with es:
        arena_t = es.enter_context(nc.sbuf_tensor("arena", [128, 53000], F32))
        A = Arena(arena_t[:, :])
        P = Prog(nc, es)
        psT = es.enter_context(nc.psum_tensor("psT", [128, 8, 128], BF16))
        banks = [es.enter_context(nc.psum_tensor("psb%d" % i, [128, 512], F32)) for i in range(7)]
        bank_rr = [0]

        def next_bank(n=7):
            i = bank_rr[0] % n
            bank_rr[0] += 1
            return i

        mod = A.alloc([6 * D], F32)
        gfb = A.alloc([D], F32)
        og = A.alloc([NT, D], BF16)
        subkT = A.alloc([16, 128], BF16)
        ident_f = A.alloc([128], F32)
        ident_b = A.alloc([128], BF16)
        ones_f = A.alloc([128], F32)
        ones_b = A.alloc([128], BF16)
        iota16 = A.alloc([16], F32)
        iota_i = A.alloc([16], I32)
        dwT = A.alloc([8, 31], F32)
        cvec = A.alloc([3, 8], F32)
        off_deadc = A.off
        sgg = A.alloc([D], F32)
        sgb = A.alloc([D], F32)
        bsp = A.alloc([8, 128], F32)
        WmT = A.alloc([8, 128], BF16)
        eps_t = A.alloc([2], F32)
        eps_ap = eps_t[:, 0:1]
        mark_global = A.off

        SH1, A1o, G1o, SH2, A2o, G2o = [i * D for i in range(6)]

        ct = A.alloc([8], F32)
        cact = A.alloc([8], F32)
        cbc = A.alloc([8, 128], BF16)
        gtmp = A.alloc([D], F32)
        wa = [A.alloc([8, 512], BF16) for _ in range(2)]
        stg = [A.alloc([8, D], BF16) for _ in range(2)]
        wsp_f = A.alloc([8, 128], F32)

        P.op("pool", lambda E: E.memset(ones_f, 1.0), writes=["ones_f"])
        P.op("pool", lambda E: E.tensor_copy(out=ones_b, in_=ones_f), reads=["ones_f"], writes=["ones_b"])
        P.op("pool", lambda E: E.affine_select(out=ident_f, in_=ones_f, pattern=[[-1, 128]],
                                               compare_op=ALU.is_equal, fill=0.0, base=0, channel_multiplier=1),
             reads=["ones_f"], writes=["ident_f"])
        P.op("pool", lambda E: E.tensor_copy(out=ident_b, in_=ident_f), reads=["ident_f"], writes=["ident_b"])
        P.op("pool", lambda E: E.iota(out=iota_i, pattern=[[1, 16]], base=0, channel_multiplier=0), writes=["iota_i"])
        P.op("pool", lambda E: E.tensor_copy(out=iota16, in_=iota_i), reads=["iota_i"], writes=["iota16"])

        P.dma("sp", lambda E: E.dma_start(out=ct, in_=c_t[:, :]), "k1", writes=["ct"])
        P.dma("sp", lambda E: E.dma_start(out=mod, in_=b_ada.partition_broadcast(128)[:, 0, :]), "k2", writes=["mod"])
        P.dma("sp", lambda E: E.dma_start(out=gfb, in_=gf.partition_broadcast(128)[:, 0, :]), "k3", writes=["gfb"])
        P.dma("pool", lambda E: E.dma_start(out=subkT, in_=subkT_d[:, :, :]), "k4", writes=["subkT"])
        P.op("act", lambda E: E.activation(out=cact, in_=ct, func=AF.Silu), reads=["ct"], writes=["cact"])
        P.op("dve", lambda E: E.tensor_copy(out=cbc, in_=cact.unsqueeze(2).to_broadcast([128, 8, 128])),
             reads=["cact"], writes=["cbc"])

        wada_v = w_ada.rearrange("(k p) n -> p k n", p=128)
        for nt in range(12):
            b = nt % 2
            P.dma("pool", lambda E, b=b, nt=nt: E.dma_start(out=wa[b], in_=wada_v[:, :, nt * 512:(nt + 1) * 512]),
                  "wa%d" % b, writes=["wa%d" % b])
            bk = next_bank()
            for k in range(8):
                P.op("pe", lambda E, b=b, k=k, bk=bk: E.matmul(banks[bk][:, :], lhsT=cbc[:, k, :], rhs=wa[b][:, k, :],
                                                               start=(k == 0), stop=(k == 7)),
                     reads=["cbc", "wa%d" % b], writes=["ps%d" % bk])
            P.op("dve", lambda E, nt=nt, bk=bk: E.tensor_tensor(out=mod[:, nt * 512:(nt + 1) * 512], in0=banks[bk][:, :],
                                                               in1=mod[:, nt * 512:(nt + 1) * 512], op=ALU.add),
                 reads=["ps%d" % bk], writes=["mod"])
        for (gd, off, sem) in ((g1, A1o, "k5"), (g2, A2o, "k6")):
            P.dma("sp", lambda E, gd=gd: E.dma_start(out=gtmp, in_=gd.partition_broadcast(128)[:, 0, :]), sem,
                  writes=["gtmp"])
            P.op("dve", lambda E, off=off: E.scalar_tensor_tensor(out=mod[:, off:off + D], in0=mod[:, off:off + D],
                                                                 scalar=1.0, in1=gtmp, op0=ALU.add, op1=ALU.mult),
                 reads=["gtmp"], writes=["mod"])

        P.dma("sp", lambda E: E.dma_start(out=dwT, in_=dwT_d[:, :, :]), "k7", writes=["dwT"])
        P.dma("sp", lambda E: E.dma_start(out=cvec, in_=cvec_d[:, :, :]), "k8", writes=["cvec"])
        P.dma("sp", lambda E: E.dma_start(out=sgg, in_=sgg_d.partition_broadcast(128)[:, 0, :]), "k9", writes=["sgg"])
        P.dma("sp", lambda E: E.dma_start(out=sgb, in_=sgb_d.partition_broadcast(128)[:, 0, :]), "k10", writes=["sgb"])
        P.dma("sp", lambda E: E.dma_start(out=bsp.rearrange("p a b -> p (a b)"),
                                          in_=bsp_d.partition_broadcast(128)[:, 0, :]), "k11", writes=["bsp"])
        P.dma("sp", lambda E: E.dma_start(out=wsp_f, in_=wspT_d[:, :, :]), "k12", writes=["wsp_f"])
        P.op("pool", lambda E: E.memset(wsp_f[64:128, :, 0:64], 0.0), writes=["wsp_f"])
        P.op("pool", lambda E: E.tensor_copy(out=WmT, in_=wsp_f), reads=["wsp_f"], writes=["WmT"])

        P.op("pool", lambda E: E.memset(eps_t, EPS), writes=["eps"])
        P.barrier(skip_prefix="c")
        A.off = mark_global

        secs = [w_in[:, i * D:(i + 1) * D] for i in range(6)] + [w_co, w_so, w_o, w_q[:, 0:D], w_q[:, D:2 * D]]
        for si, wsec in enumerate(secs):
            P.dma("pool", lambda E, si=si, wsec=wsec: E.dma_start(out=wsc[si], in_=wsec.rearrange("(k p) n -> p k n", p=128)),
                  "cw%d" % si, writes=["wsc%d" % si])
        NCV = 16
        for ci_ in range(NCV):
            r0 = ci_ * (16384 // NCV)
            P.dma("pool", lambda E, r0=r0: E.dma_start(
                out=uvb[r0:r0 + 16384 // NCV, :].rearrange("(a b) n -> a b n", a=8),
                in_=uv[r0:r0 + 16384 // NCV, :].rearrange("(a b) n -> a b n", a=8)),
                "cv%d" % ci_, writes=["uvb"])
        wring = [A.alloc([8, D], BF16) for _ in range(3)]
        xt = [A.alloc([D], F32) for _ in range(2)]
        nb = [A.alloc([D], BF16) for _ in range(2)]
        nT = A.alloc([8, TT], BF16)
        z = A.alloc([8, 30 + TT], BF16)
        acc = A.alloc([8, TT], F32)
        sq = A.alloc([8, TT], BF16)
        zs = A.alloc([8, TT], BF16)
        uT = A.alloc([8, TT], BF16)
        vtmp = A.alloc([D], F32)
        na = vtmp
        vn = [A.alloc([D], BF16) for _ in range(2)]
        sgA = A.alloc([8, TT], BF16)
        sgB = A.alloc([8, TT], BF16)
        gatedT = A.alloc([8, TT], BF16)
        m1 = A.alloc([8, TT], BF16)
        m2 = A.alloc([TT], F32)
        mergedT = A.alloc([8, TT], BF16)
        sgt = A.alloc([TT], F32)
        tmpg = A.alloc([TT], F32)
        mean_c = A.alloc([TT], F32)
        msq_c = A.alloc([TT], F32)
        rstd_c = A.alloc([TT], F32)
        small = A.alloc([64], F32)
        NDG = 12
        dgr = [A.alloc([128], BF16) for _ in range(NDG)]
        dgc = [0]
        print("phase1 arena words", A.off)

        P.op("dve", lambda E: E.memset(z[:, :, 0:30], 0.0), writes=["z%d" % c for c in range(8)])
        seq = [(tt, sec) for tt in range(NTT) for sec in range(9)]

        def issue_wload(n):
            if n >= len(seq):
                return
            _, sec = seq[n]
            sl = n % 3
            P.dma("sp", lambda E, sl=sl, sec=sec: E.dma_start(out=wring[sl], in_=wsc[sec]), "wr%d" % sl,
                  reads=["wsc%d" % sec], writes=["wr%d" % sl])

        issue_wload(0)
        issue_wload(1)

        def mm8(out_ap, lhs_fn, rhs_fn, reads, bk):
            for k in range(8):
                P.op("pe", lambda E, k=k: E.matmul(out_ap, lhsT=lhs_fn(k), rhs=rhs_fn(k), start=(k == 0), stop=(k == 7)),
                     reads=reads, writes=["ps%d" % bk])

        def rms_prep(src, sskey, pre, junk, junkkey):
            P.op("act", lambda E: E.activation(out=junk, in_=src, func=AF.Square, accum_out=small[:, pre:pre + 1]),
                 reads=[sskey], writes=[junkkey, "sm%d" % pre])
            P.op("act", lambda E: E.activation(out=small[:, pre + 1:pre + 2], in_=small[:, pre:pre + 1], func=AF.Sqrt,
                                               scale=1.0 / D, bias=eps_ap),
                 reads=["sm%d" % pre, "eps"], writes=["sm%d" % (pre + 1)])
            P.op("dve", lambda E: E.reciprocal(out=small[:, pre + 2:pre + 3], in_=small[:, pre + 1:pre + 2]),
                 reads=["sm%d" % (pre + 1)], writes=["sm%d" % (pre + 2)])


        for tt in range(NTT):
            n0 = tt * 9
            for s in range(2):
                if tt == 0:
                    P.dma("sp", lambda E, s=s: E.dma_start(out=xt[s], in_=x[s * 128:(s + 1) * 128, :]), "xt%d" % s,
                          writes=["xt%d" % s])
                pre = 4 * s
                rms_prep(xt[s], "xt%d" % s, pre, nb[s], "nb%d" % s)
                P.op("dve", lambda E, s=s, pre=pre: E.scalar_tensor_tensor(
                    out=na, in0=xt[s], scalar=small[:, pre + 2:pre + 3], in1=mod[:, A1o:A1o + D],
                    op0=ALU.mult, op1=ALU.mult), reads=["xt%d" % s, "sm%d" % (pre + 2), "mod"], writes=["vtmp"])
                P.op("dve", lambda E, s=s: E.tensor_tensor(out=nb[s], in0=na, in1=mod[:, SH1:SH1 + D], op=ALU.add),
                     reads=["vtmp", "mod"], writes=["nb%d" % s])
                if tt + 1 < NTT:
                    tokn = (tt + 1) * TT + s * 128
                    P.dma("sp", lambda E, s=s, tokn=tokn: E.dma_start(out=xt[s], in_=x[tokn:tokn + 128, :]), "xt%d" % s,
                          writes=["xt%d" % s])
                for k in range(8):
                    P.op("pe", lambda E, s=s, k=k: E.transpose(out=psT[:, k, :], in_=nb[s][:, k * 128:(k + 1) * 128],
                                                               identity=ident_b),
                         reads=["nb%d" % s, "ident_b"], writes=["psT"])
                P.op("act", lambda E, s=s: E.copy(out=nT[:, :, s * 128:(s + 1) * 128], in_=psT[:, :, :]),
                     reads=["psT"], writes=["nT"])

            issue_wload(n0 + 2)
            w0, w1 = wring[(n0) % 3], wring[(n0 + 1) % 3]
            k0, k1 = "wr%d" % (n0 % 3), "wr%d" % ((n0 + 1) % 3)
            issue_wload(n0 + 3) if False else None
            for c in range(8):
                bk = next_bank()
                mm8(banks[bk][:, 0:TT], lambda k, c=c: w0[:, k, c * 128:(c + 1) * 128], lambda k: nT[:, k, :],
                    [k0, "nT"], bk)
                mm8(banks[bk][:, TT:2 * TT], lambda k, c=c: w1[:, k, c * 128:(c + 1) * 128], lambda k: nT[:, k, :],
                    [k1, "nT"], bk)
                P.op("act", lambda E, bk=bk: E.activation(out=sgt, in_=banks[bk][:, TT:2 * TT], func=AF.Sigmoid),
                     reads=["ps%d" % bk], writes=["sgt"])
                P.op("dve", lambda E, bk=bk, c=c: E.tensor_tensor(out=z[:, c, 30:30 + TT], in0=banks[bk][:, 0:TT],
                                                                 in1=sgt, op=ALU.mult),
                     reads=["ps%d" % bk, "sgt"], writes=["z%d" % c])
            issue_wload(n0 + 3)
            w2, k2 = wring[(n0 + 2) % 3], "wr%d" % ((n0 + 2) % 3)
            for c in range(8):
                bk = next_bank()
                mm8(banks[bk][:, 0:TT], lambda k, c=c: w2[:, k, c * 128:(c + 1) * 128], lambda k: nT[:, k, :],
                    [k2, "nT"], bk)
                P.op("act", lambda E, bk=bk, c=c: E.copy(out=uT[:, c, :], in_=banks[bk][:, 0:TT]),
                     reads=["ps%d" % bk], writes=["uT%d" % c])
            issue_wload(n0 + 4)
            w3, k3 = wring[(n0 + 3) % 3], "wr%d" % ((n0 + 3) % 3)
            for s in range(2):
                bks = [next_bank(), next_bank()]
                for nh in range(2):
                    mm8(banks[bks[nh]][:, :], lambda k, s=s: nT[:, k, s * 128:(s + 1) * 128],
                        lambda k, nh=nh: w3[:, k, nh * 512:(nh + 1) * 512], [k3, "nT"], bks[nh])
                    P.op("dve", lambda E, nh=nh, bks=bks: E.bn_stats(out=small[:, 16 + 6 * nh:22 + 6 * nh],
                                                                    in_=banks[bks[nh]][:, :]),
                         reads=["ps%d" % bks[nh]], writes=["bst%d" % nh])
                P.op("dve", lambda E: E.bn_aggr(out=small[:, 28:30], in_=small[:, 16:28]),
                     reads=["bst0", "bst1"], writes=["mv"])
                P.op("act", lambda E: E.activation(out=small[:, 30:31], in_=small[:, 29:30], func=AF.Sqrt, bias=eps_ap,
                                                   scale=1.0), reads=["mv", "eps"], writes=["vsd"])
                P.op("dve", lambda E: E.reciprocal(out=small[:, 31:32], in_=small[:, 30:31]), reads=["vsd"],
                     writes=["vrs"])
                for nh in range(2):
                    P.op("dve", lambda E, nh=nh, bks=bks: E.tensor_scalar(
                        out=vtmp[:, nh * 512:(nh + 1) * 512], in0=banks[bks[nh]][:, :], scalar1=small[:, 28:29],
                        scalar2=small[:, 31:32], op0=ALU.subtract, op1=ALU.mult),
                        reads=["ps%d" % bks[nh], "mv", "vrs"], writes=["vtmp"])
                P.op("dve", lambda E: E.tensor_tensor(out=vtmp, in0=vtmp, in1=sgg, op=ALU.mult),
                     reads=["sgg"], writes=["vtmp"])
                P.op("dve", lambda E, s=s: E.tensor_tensor(out=vn[s], in0=vtmp, in1=sgb, op=ALU.add),
                     reads=["vtmp", "sgb"], writes=["vn%d" % s])
            for g in range(8):
                bk = next_bank()
                for blk in range(2):
                    P.op("pe", lambda E, g=g, blk=blk, bk=bk: E.matmul(
                        banks[bk][:, blk * 128:(blk + 1) * 128], lhsT=vn[blk][:, g * 128:(g + 1) * 128], rhs=WmT[:, g, :],
                        start=True, stop=True), reads=["vn%d" % blk, "WmT"], writes=["ps%d" % bk])
                P.op("dve", lambda E, g=g, bk=bk: E.tensor_tensor(
                    out=tmpg.rearrange("p (a b) -> p a b", a=2), in0=banks[bk][:, 0:TT].rearrange("p (a b) -> p a b", a=2),
                    in1=bsp[:, g, :].unsqueeze(1).to_broadcast([128, 2, 128]), op=ALU.add),
                    reads=["ps%d" % bk, "bsp"], writes=["tmpg"])
                P.op("dve", lambda E, g=g: E.tensor_tensor(out=gatedT[:, g, :], in0=tmpg, in1=uT[:, g, :], op=ALU.mult),
                     reads=["tmpg", "uT%d" % g], writes=["gT%d" % g])
            for c in range(8):
                bk = next_bank()
                for w in range(31):
                    dsl = dgc[0] % NDG
                    dgc[0] += 1
                    if w % 3 == 2:
                        P.op("act", lambda E, c=c, w=w, dsl=dsl: E.activation(
                            out=dgr[dsl], in_=ident_b, func=AF.Copy, scale=dwT[:, c, w:w + 1]),
                            reads=["ident_b", "dwT"], writes=["dg%d" % dsl])
                    else:
                        P.op("dve", lambda E, c=c, w=w, dsl=dsl: E.tensor_scalar(
                            out=dgr[dsl], in0=ident_b, scalar1=dwT[:, c, w:w + 1], scalar2=None, op0=ALU.mult),
                            reads=["ident_b", "dwT"], writes=["dg%d" % dsl])
                    P.op("pe", lambda E, c=c, w=w, dsl=dsl, bk=bk: E.matmul(
                        banks[bk][:, 0:TT], lhsT=dgr[dsl], rhs=z[:, c, w:w + TT], start=(w == 0), stop=(w == 30)),
                        reads=["dg%d" % dsl, "z%d" % c], writes=["ps%d" % bk])
                P.op("act", lambda E, c=c, bk=bk: E.activation(out=acc[:, c, :], in_=banks[bk][:, 0:TT], func=AF.Identity,
                                                               bias=cvec[:, 0, c:c + 1]),
                     reads=["ps%d" % bk, "cvec"], writes=["acc%d" % c])
                P.op("act", lambda E, c=c: E.copy(out=z[:, c, 0:30], in_=z[:, c, TT:TT + 30]),
                     reads=["z%d" % c], writes=["z%d" % c])
                P.op("act", lambda E, c=c: E.activation(out=sq[:, c, :], in_=acc[:, c, :], func=AF.Square),
                     reads=["acc%d" % c], writes=["sq%d" % c])
            for (sidx, dst, nm) in ((4, sgA, "sgA"), (5, sgB, "sgB")):
                issue_wload(n0 + sidx + 1)
                wS, kS = wring[(n0 + sidx) % 3], "wr%d" % ((n0 + sidx) % 3)
                for c in range(8):
                    bk = next_bank()
                    mm8(banks[bk][:, 0:TT], lambda k, c=c, wS=wS: wS[:, k, c * 128:(c + 1) * 128], lambda k: nT[:, k, :],
                        [kS, "nT"], bk)
                    P.op("act", lambda E, bk=bk, c=c, dst=dst: E.activation(out=dst[:, c, :], in_=banks[bk][:, 0:TT],
                                                                           func=AF.Sigmoid),
                         reads=["ps%d" % bk], writes=["%s%d" % (nm, c)])
            bkS = next_bank()
            for c in range(8):
                P.op("pe", lambda E, c=c, bkS=bkS: E.matmul(banks[bkS][:, 0:TT], lhsT=ones_f, rhs=acc[:, c, :],
                                                   start=(c == 0), stop=(c == 7)),
                     reads=["ones_f", "acc%d" % c], writes=["ps%d" % bkS])
            for c in range(8):
                P.op("pe", lambda E, c=c, bkS=bkS: E.matmul(banks[bkS][:, TT:2 * TT], lhsT=ones_b, rhs=sq[:, c, :],
                                                   start=(c == 0), stop=(c == 7)),
                     reads=["ones_b", "sq%d" % c], writes=["ps%d" % bkS])
            P.op("dve", lambda E, bkS=bkS: E.tensor_scalar(out=mean_c, in0=banks[bkS][:, 0:TT], scalar1=1.0 / D, scalar2=None,
                                                  op0=ALU.mult), reads=["ps%d" % bkS], writes=["mean_c"])
            P.op("dve", lambda E: E.tensor_tensor(out=msq_c, in0=mean_c, in1=mean_c, op=ALU.mult),
                 reads=["mean_c"], writes=["msq_c"])
            P.op("dve", lambda E, bkS=bkS: E.scalar_tensor_tensor(out=msq_c, in0=banks[bkS][:, TT:2 * TT], scalar=1.0 / D,
                                                         in1=msq_c, op0=ALU.mult, op1=ALU.subtract),
                 reads=["ps%d" % bkS, "msq_c"], writes=["msq_c"])
            P.op("act", lambda E: E.activation(out=rstd_c, in_=msq_c, func=AF.Sqrt, bias=eps_ap, scale=1.0),
                 reads=["msq_c", "eps"], writes=["rstd_c"])
            P.op("dve", lambda E: E.reciprocal(out=rstd_c, in_=rstd_c), reads=["rstd_c"], writes=["rstd_c"])
            for c in range(8):
                P.op("dve", lambda E, c=c: E.tensor_tensor(out=acc[:, c, :], in0=acc[:, c, :], in1=mean_c,
                                                          op=ALU.subtract),
                     reads=["mean_c"], writes=["acc%d" % c])
                P.op("dve", lambda E, c=c: E.tensor_tensor(out=acc[:, c, :], in0=acc[:, c, :], in1=rstd_c, op=ALU.mult),
                     reads=["rstd_c"], writes=["acc%d" % c])
                P.op("act", lambda E, c=c: E.activation(out=zs[:, c, :], in_=acc[:, c, :], func=AF.Silu,
                                                        scale=cvec[:, 1, c:c + 1], bias=cvec[:, 2, c:c + 1]),
                     reads=["acc%d" % c, "cvec"], writes=["zs%d" % c])
            issue_wload(n0 + 7)
            w6, k6 = wring[(n0 + 6) % 3], "wr%d" % ((n0 + 6) % 3)
            for dc in range(8):
                bk = next_bank()
                for c in range(8):
                    P.op("pe", lambda E, c=c, dc=dc, bk=bk: E.matmul(banks[bk][:, 0:TT], lhsT=w6[:, c, dc * 128:(dc + 1) * 128],
                                                                    rhs=zs[:, c, :], start=(c == 0), stop=(c == 7)),
                         reads=[k6, "zs%d" % c], writes=["ps%d" % bk])
                P.op("dve", lambda E, dc=dc, bk=bk: E.tensor_tensor(out=m1[:, dc, :], in0=banks[bk][:, 0:TT],
                                                                   in1=sgA[:, dc, :], op=ALU.mult),
                     reads=["ps%d" % bk, "sgA%d" % dc], writes=["m1_%d" % dc])
            issue_wload(n0 + 8)
            w7, k7 = wring[(n0 + 7) % 3], "wr%d" % ((n0 + 7) % 3)
            for dc in range(8):
                bk = next_bank()
                for c in range(8):
                    P.op("pe", lambda E, c=c, dc=dc, bk=bk: E.matmul(banks[bk][:, 0:TT], lhsT=w7[:, c, dc * 128:(dc + 1) * 128],
                                                                    rhs=gatedT[:, c, :], start=(c == 0), stop=(c == 7)),
                         reads=[k7, "gT%d" % c], writes=["ps%d" % bk])
                P.op("dve", lambda E, dc=dc, bk=bk: E.tensor_tensor(out=m2, in0=banks[bk][:, 0:TT], in1=sgB[:, dc, :],
                                                                   op=ALU.mult),
                     reads=["ps%d" % bk, "sgB%d" % dc], writes=["m2"])
                P.op("dve", lambda E, dc=dc: E.tensor_tensor(out=mergedT[:, dc, :], in0=m1[:, dc, :], in1=m2, op=ALU.add),
                     reads=["m1_%d" % dc, "m2"], writes=["mg%d" % dc])
            issue_wload(n0 + 9)
            issue_wload(n0 + 10)
            w8, k8 = wring[(n0 + 8) % 3], "wr%d" % ((n0 + 8) % 3)
            for s in range(2):
                tile = tt * 2 + s
                for nh in range(2):
                    bk = next_bank()
                    for dc in range(8):
                        P.op("pe", lambda E, s=s, nh=nh, dc=dc, bk=bk: E.matmul(
                            banks[bk][:, :], lhsT=mergedT[:, dc, s * 128:(s + 1) * 128],
                            rhs=w8[:, dc, nh * 512:(nh + 1) * 512], start=(dc == 0), stop=(dc == 7)),
                            reads=[k8, "mg%d" % dc], writes=["ps%d" % bk])
                    P.op("dve", lambda E, tile=tile, nh=nh, bk=bk: E.tensor_tensor(
                        out=og[:, tile, nh * 512:(nh + 1) * 512], in0=banks[bk][:, :],
                        in1=mod[:, G1o + nh * 512:G1o + (nh + 1) * 512], op=ALU.mult),
                        reads=["ps%d" % bk, "mod"], writes=["og%d" % tile])

        if debug == "og":
            P.dma("sp", lambda E: E.dma_start(out=dbg[:, 0:2 * NTT, :], in_=og[:, 0:2 * NTT, :]), "dbg",
                  reads=["og%d" % t for t in range(2 * NTT)])
            P.finish(["dbg"])
            P.emit()
            return nc

        P.barrier(skip_prefix="c")
        A.off = mark_global

        IOA = bass.IndirectOffsetOnAxis
        wq = A.alloc([8, 2 * D], BF16)
        def _bf(ap_f32, nslots):
            return ap_f32.bitcast(BF16).rearrange("p (a b) -> p a b", a=nslots)
        arena_ap = arena_t[:, :]
        ringb = []
        for _ in range(3):
            t_ = A.alloc([BATCH, 2 * D], BF16)
            ringb.append(([t_[:, jj, :] for jj in range(BATCH)], t_[:, 2:4, :]))
        deadc = arena_ap[:, off_deadc:off_deadc + 3072]
        pair5 = _bf(deadc[:, 0:2048], 2)
        s5a = _bf(deadc[:, 2048:3072], 1)
        s5b = _bf(mod[:, 1024:2048], 1)
        ringb.append(([s5a[:, 0, :], s5b[:, 0, :], pair5[:, 0, :], pair5[:, 1, :]], pair5))
        NRB = len(ringb)
        prod = _bf(mod[:, 0:1024], 2)
        junkA = mod[:, 2048:2560].bitcast(BF16)
        junkD = A.alloc([D], BF16)
        n2T = mod[:, 2560:3072].bitcast(BF16).rearrange("p (a b) -> p a b", a=8)
        h1 = [A.alloc([D], F32) for _ in range(3)]
        n2b = [A.alloc([D], BF16) for _ in range(2)]
        eidx = [A.alloc([8, 16], I32) for _ in range(2)]
        gate = [A.alloc([8, 16], F32) for _ in range(2)]
        n2 = A.alloc([D], F32)
        h2 = [n2, n2]
        vals = A.alloc([8, 2, 16], F32)
        idxs = A.alloc([8, 2, 16], U32)
        tmpm = [A.alloc([128], F32) for _ in range(2)]
        cand = A.alloc([8, 16, 16], F32)
        qT = cand.rearrange("p a b c -> p (a b c)")[:, 0:1024].bitcast(BF16).rearrange("p (a b) -> p a b", a=16)
        tmpc = [A.alloc([256], F32) for _ in range(2)]
        st = A.alloc([8, 16], F32)
        ci = A.alloc([8, 16], U32)
        ar = A.alloc([8, 16], U32)
        br = A.alloc([8, 16], U32)
        eq = cand
        i1sel = A.alloc([8, 16], F32)
        i2sel = A.alloc([8, 16], F32)
        ex = A.alloc([8, 16], F32)
        ssum = A.alloc([8], F32)
        av = A.alloc([NSLOT], F32)
        gsg = [A.alloc([BATCH], F32) for _ in range(4)]
        hg = A.alloc([NSLOT], F32)
        NDIAG = 2 * BATCH
        diag = [A.alloc([128], BF16) for _ in range(NDIAG)]
        sm2 = A.alloc([64], F32)
        print("phase2 arena words", A.off)


        for half in range(2):
            P.dma("sp", lambda E, half=half: E.dma_start(out=wq[:, :, half * D:(half + 1) * D], in_=wsc[9 + half]),
                  "wq%d" % half, reads=["wsc%d" % (9 + half)], writes=["wq"])

        def rms2(src, sskey, pre, jk, jkkey):
            P.op("act", lambda E: E.activation(out=jk, in_=src, func=AF.Square, accum_out=sm2[:, pre:pre + 1]),
                 reads=[sskey], writes=[jkkey, "s2_%d" % pre])
            P.op("act", lambda E: E.activation(out=sm2[:, pre + 1:pre + 2], in_=sm2[:, pre:pre + 1], func=AF.Sqrt,
                                               scale=1.0 / D, bias=eps_ap),
                 reads=["s2_%d" % pre, "eps"], writes=["s2_%d" % (pre + 1)])
            P.op("dve", lambda E: E.reciprocal(out=sm2[:, pre + 2:pre + 3], in_=sm2[:, pre + 1:pre + 2]),
                 reads=["s2_%d" % (pre + 1)], writes=["s2_%d" % (pre + 2)])

        def sc_ap(hp):
            return banks[hp // 4][:, (hp % 4) * 128:(hp % 4 + 1) * 128]

        def top16_multi(chains):
            for stage in range(5):
                for (src_fn, srckeys, vout_fn, iout_fn, vkey, ikey, tmpbuf, tmpkey) in chains:
                    if stage == 0:
                        P.op("dve", lambda E, src_fn=src_fn, vout_fn=vout_fn: E.max(out=vout_fn(0), in_=src_fn()),
                             reads=srckeys, writes=[vkey + "a"])
                    elif stage == 1:
                        P.op("dve", lambda E, src_fn=src_fn, vout_fn=vout_fn, iout_fn=iout_fn: E.max_index(
                            out=iout_fn(0), in_max=vout_fn(0), in_values=src_fn()),
                            reads=srckeys + [vkey + "a"], writes=[ikey + "a"])
                    elif stage == 2:
                        P.op("dve", lambda E, src_fn=src_fn, vout_fn=vout_fn, tmpbuf=tmpbuf: E.match_replace(
                            out=tmpbuf, in_to_replace=vout_fn(0), in_values=src_fn(), imm_value=-1e30),
                            reads=srckeys + [vkey + "a"], writes=[tmpkey])
                    elif stage == 3:
                        P.op("dve", lambda E, vout_fn=vout_fn, tmpbuf=tmpbuf: E.max(out=vout_fn(1), in_=tmpbuf),
                             reads=[tmpkey], writes=[vkey + "b"])
                    else:
                        P.op("dve", lambda E, vout_fn=vout_fn, iout_fn=iout_fn, tmpbuf=tmpbuf: E.max_index(
                            out=iout_fn(1), in_max=vout_fn(1), in_values=tmpbuf),
                            reads=[tmpkey, vkey + "b"], writes=[ikey + "b"])

        def prep_load(i):
            b3 = i % 3
            P.dma("sp", lambda E: E.dma_start(out=h1[b3], in_=x[i * 128:(i + 1) * 128, :]), "h1_%d" % b3,
                  writes=["h1_%d" % b3])

        def prep(i):
            b = i % 2
            b3 = i % 3
            pre = 4 * b
            P.op("dve", lambda E: E.tensor_tensor(out=h1[b3], in0=h1[b3], in1=og[:, i, :], op=ALU.add),
                 reads=["og%d" % i], writes=["h1_%d" % b3])
            yield "x"
            P.op("act", lambda E: E.activation(out=junkA, in_=h1[b3], func=AF.Square, accum_out=sm2[:, pre:pre + 1]),
                 reads=["h1_%d" % b3], writes=["junkA", "s2_%d" % pre])
            P.op("act", lambda E: E.activation(out=sm2[:, pre + 1:pre + 2], in_=sm2[:, pre:pre + 1], func=AF.Sqrt,
                                               scale=1.0 / D, bias=eps_ap),
                 reads=["s2_%d" % pre, "eps"], writes=["s2_%d" % (pre + 1)])
            yield "x"
            P.op("dve", lambda E: E.reciprocal(out=sm2[:, pre + 2:pre + 3], in_=sm2[:, pre + 1:pre + 2]),
                 reads=["s2_%d" % (pre + 1)], writes=["s2_%d" % (pre + 2)])
            P.op("dve", lambda E: E.scalar_tensor_tensor(out=n2, in0=h1[b3], scalar=sm2[:, pre + 2:pre + 3],
                                                         in1=mod[:, A2o:A2o + D], op0=ALU.mult, op1=ALU.mult),
                 reads=["h1_%d" % b3, "s2_%d" % (pre + 2), "mod"], writes=["n2"])
            P.op("dve", lambda E: E.tensor_tensor(out=n2b[b], in0=n2, in1=mod[:, SH2:SH2 + D], op=ALU.add),
                 reads=["n2", "mod"], writes=["n2b%d" % b])
            yield "x"
            for k in range(8):
                P.op("pe", lambda E, k=k: E.transpose(out=psT[:, k, :], in_=n2b[b][:, k * 128:(k + 1) * 128],
                                                      identity=ident_b),
                     reads=["n2b%d" % b, "ident_b"], writes=["psT"])
            yield "x"
            P.op("act", lambda E: E.copy(out=n2T, in_=psT[:, :, :]), reads=["psT"], writes=["n2T"])
            yield "x"
            for gq in range(4):
                for jj in range(4):
                    hp = gq * 4 + jj
                    mm8(banks[6][:, jj * 128:(jj + 1) * 128], lambda k, hp=hp: wq[:, k, hp * 128:(hp + 1) * 128],
                        lambda k: n2T[:, k, :], ["wq", "n2T"], 6)
                yield "x"
                P.op("act", lambda E, gq=gq: E.copy(out=qT[:, gq * 4:(gq + 1) * 4, :],
                                                    in_=banks[6][:, :].rearrange("p (a b) -> p a b", a=4)),
                     reads=["ps6"], writes=["cand"])
                yield "x"
            for hp in range(16):
                P.op("pe", lambda E, hp=hp: E.matmul(sc_ap(hp), lhsT=qT[:, hp, :], rhs=subkT[:, hp, :], start=True,
                                                     stop=True),
                     reads=["cand", "subkT"], writes=["ps%d" % (hp // 4)])
            yield "x"
            for hp0 in range(0, 16, 2):
                chains = []
                for hp in (hp0, hp0 + 1):
                    h, p = hp // 2, hp % 2
                    chains.append((lambda hp=hp: sc_ap(hp), ["ps%d" % (hp // 4)],
                                   lambda r, h=h, p=p: vals[:, h, p, 8 * r:8 * r + 8],
                                   lambda r, h=h, p=p: idxs[:, h, p, 8 * r:8 * r + 8],
                                   "vals%d" % hp, "idxs%d" % hp, tmpm[hp % 2], "tmpm%d" % (hp % 2)))
                top16_multi(chains)
                yield "d"
            P.op("dve", lambda E: E.tensor_tensor(
                out=cand, in0=vals[:, :, 0, :].unsqueeze(3).to_broadcast([128, 8, 16, 16]),
                in1=vals[:, :, 1, :].unsqueeze(2).to_broadcast([128, 8, 16, 16]), op=ALU.add),
                reads=["vals%d%s" % (hp, ab) for hp in range(16) for ab in "ab"], writes=["cand"])
            for h0 in range(0, 8, 2):
                chains = []
                for h in (h0, h0 + 1):
                    chains.append((lambda h=h: cand[:, h, :, :].rearrange("p a b -> p (a b)"), ["cand"],
                                   lambda r, h=h: st[:, h, 8 * r:8 * r + 8], lambda r, h=h: ci[:, h, 8 * r:8 * r + 8],
                                   "st%d" % h, "ci%d" % h, tmpc[h % 2], "tmpc%d" % (h % 2)))
                top16_multi(chains)
                yield "d"
            P.op("dve", lambda E: E.tensor_single_scalar(out=ar, in_=ci, scalar=4, op=ALU.logical_shift_right),
                 reads=["ci%d%s" % (h, ab) for h in range(8) for ab in "ab"], writes=["ar"])
            P.op("dve", lambda E: E.tensor_single_scalar(out=br, in_=ci, scalar=15, op=ALU.bitwise_and),
                 reads=["ci%d%s" % (h, ab) for h in range(8) for ab in "ab"], writes=["br"])
            for (sel, src, pp, key) in ((i1sel, ar, 0, "i1sel"), (i2sel, br, 1, "i2sel")):
                P.op("dve", lambda E, src=src: E.tensor_tensor(
                    out=eq, in0=src.unsqueeze(3).to_broadcast([128, 8, 16, 16]),
                    in1=iota16.unsqueeze(1).unsqueeze(1).to_broadcast([128, 8, 16, 16]), op=ALU.is_equal),
                    reads=["ar", "br", "iota16"], writes=["cand"])
                P.op("dve", lambda E, pp=pp: E.tensor_tensor(
                    out=eq, in0=eq, in1=idxs[:, :, pp, :].unsqueeze(2).to_broadcast([128, 8, 16, 16]), op=ALU.mult),
                    reads=["idxs%d%s" % (hp, ab) for hp in range(16) for ab in "ab"], writes=["cand"])
                P.op("dve", lambda E, sel=sel: E.tensor_reduce(out=sel, in_=eq, axis=AX.X, op=ALU.add),
                     reads=["cand"], writes=[key])
                yield "d"
            P.op("dve", lambda E: E.scalar_tensor_tensor(out=eidx[b], in0=i1sel, scalar=128.0, in1=i2sel,
                                                         op0=ALU.mult, op1=ALU.add),
                 reads=["i1sel", "i2sel"], writes=["eidx%d" % b])
            P.op("dve", lambda E: E.tensor_tensor(out=ex, in0=st, in1=st[:, :, 0:1].to_broadcast([128, 8, 16]),
                                                  op=ALU.subtract), reads=["st%d%s" % (h, ab) for h in range(8) for ab in "ab"], writes=["ex"])
            yield "x"
            P.op("act", lambda E: E.activation(out=ex, in_=ex, func=AF.Exp), reads=["ex"], writes=["ex"])
            yield "x"
            P.op("dve", lambda E: E.tensor_reduce(out=ssum, in_=ex, axis=AX.X, op=ALU.add), reads=["ex"],
                 writes=["ssum"])
            P.op("dve", lambda E: E.reciprocal(out=ssum, in_=ssum), reads=["ssum"], writes=["ssum"])
            P.op("dve", lambda E: E.tensor_tensor(out=gate[b], in0=ex, in1=ssum.unsqueeze(2).to_broadcast([128, 8, 16]),
                                                  op=ALU.mult), reads=["ex", "ssum"], writes=["gate%d" % b])
            yield "x"

        gcount = [0]
        breg = [None]
        P.stream["pool"].append(("raw", lambda E: breg.__setitem__(0, E.to_reg(16383))))

        def tile_fns(i):
            b = i % 2
            eflat = eidx[b].rearrange("p a b -> p (a b)")
            gflat = gate[b].rearrange("p a b -> p (a b)")
            NSTT = 2

            def gathers(j0):
                rb = gcount[0] % NRB
                gcount[0] += 1
                rslots, rpair = ringb[rb]
                for jj in range(BATCH):
                    j = j0 + jj
                    P.dma("pool", lambda E, j=j, jj=jj, rslots=rslots: E.indirect_dma_start(
                        out=rslots[jj], out_offset=None, in_=uvb[:, :], in_offset=IOA(ap=eflat[:, j:j + 1], axis=0),
                        bounds_check=breg[0], oob_is_err=False),
                        "g%d_%d" % (rb, jj), reads=["eidx%d" % b, "uvb"], writes=["ring%d_%d" % (rb, jj)])
                return (j0, rb, rslots, rpair)

            def products(ctx):
                j0, rb, rslots, rpair = ctx
                rkeys = ["ring%d_%d" % (rb, jj) for jj in range(BATCH)]
                for jj in range(NSTT):
                    j = j0 + jj
                    P.op("dve", lambda E, rslots=rslots, j=j, jj=jj: E.scalar_tensor_tensor(
                        out=junkD, in0=rslots[jj][:, 0:D], scalar=1.0, in1=n2b[b], op0=ALU.mult, op1=ALU.mult,
                        accum_out=av[:, j:j + 1]),
                        reads=[rkeys[jj], "n2b%d" % b], writes=["junkD", "av%d" % j])
                P.op("dve", lambda E, rpair=rpair: E.tensor_tensor(
                    out=prod, in0=rpair[:, :, 0:D],
                    in1=n2b[b].unsqueeze(1).to_broadcast([128, BATCH - NSTT, D]), op=ALU.mult),
                    reads=rkeys[NSTT:] + ["n2b%d" % b], writes=["prod"])

            def reduces(ctx):
                j0 = ctx[0]
                for jj in range(NSTT, BATCH):
                    j = j0 + jj
                    P.op("act", lambda E, j=j, jj=jj: E.activation(out=junkA, in_=prod[:, jj - NSTT, :], func=AF.Copy,
                                                                   accum_out=av[:, j:j + 1]),
                         reads=["prod"], writes=["junkA", "av%d" % j])

            def tail_gelu(ctx):
                j0 = ctx[0]
                akeys = ["av%d" % j for j in range(j0, j0 + BATCH)]
                gs = gsg[(j0 // BATCH) % 4]
                gk = "gsg%d" % ((j0 // BATCH) % 4)
                P.op("act", lambda E: E.activation(out=gs, in_=av[:, j0:j0 + BATCH], func=AF.Gelu_apprx_tanh),
                     reads=akeys, writes=[gk])

            def tail_rest(ctx):
                j0, rb, rslots, rpair = ctx
                gs = gsg[(j0 // BATCH) % 4]
                gk = "gsg%d" % ((j0 // BATCH) % 4)
                P.op("dve", lambda E: E.tensor_tensor(out=hg[:, j0:j0 + BATCH], in0=gs, in1=gflat[:, j0:j0 + BATCH],
                                                      op=ALU.mult),
                     reads=[gk, "gate%d" % b], writes=["hg%d" % (j0 // BATCH)])
                for jj, j in enumerate(range(j0, j0 + BATCH)):
                    ds = j % NDIAG
                    P.op("act", lambda E, j=j, ds=ds: E.activation(out=diag[ds], in_=ident_b, func=AF.Copy,
                                                                   scale=hg[:, j:j + 1]),
                         reads=["ident_b", "hg%d" % (j0 // BATCH)], writes=["diag%d" % ds])
                    for half in range(2):
                        P.op("pe", lambda E, j=j, ds=ds, jj=jj, half=half, rslots=rslots: E.matmul(
                            banks[4 + half][:, :], lhsT=diag[ds], rhs=rslots[jj][:, D + half * 512:D + (half + 1) * 512],
                            start=(j == 0), stop=(j == NSLOT - 1)),
                            reads=["diag%d" % ds, "ring%d_%d" % (rb, jj)], writes=["ps%d" % (4 + half)])

            return gathers, products, reduces, tail_gelu, tail_rest

        HOIST = 2

        def compute(i, mid_hook, pre_ctxs, has_next):
            b = i % 2
            gathers, products, reduces, tail_gelu, tail_rest = tile_fns(i)
            ctxs = list(pre_ctxs)
            npre = len(ctxs)
            nb_ = NSLOT // BATCH
            for kb in range(nb_ + 1):
                if mid_hook is not None and kb >= 1:
                    tag = next(mid_hook, None)
                    if tag == "d" and kb >= 28:
                        next(mid_hook, None)
                if npre <= kb < nb_:
                    ctxs.append(gathers(kb * BATCH))
                if kb >= 1:
                    tail_gelu(ctxs[kb - 1])
                if npre <= kb < nb_:
                    products(ctxs[kb])
                    reduces(ctxs[kb])
                if kb >= 1:
                    tail_rest(ctxs[kb - 1])
            if mid_hook is not None:
                for _ in mid_hook:
                    pass
            nxt = []
            if has_next:
                ng, np_, nr, _, _ = tile_fns(i + 1)
                for kb in range(HOIST):
                    c_ = ng(kb * BATCH)
                    np_(c_)
                    nr(c_)
                    nxt.append(c_)
            for half in range(2):
                P.op("dve", lambda E, half=half: E.tensor_tensor(
                    out=h2[b][:, half * 512:(half + 1) * 512], in0=banks[4 + half][:, :],
                    in1=mod[:, G2o + half * 512:G2o + (half + 1) * 512], op=ALU.mult),
                    reads=["ps%d" % (4 + half), "mod"], writes=["n2"])
            P.op("dve", lambda E: E.tensor_tensor(out=h2[b], in0=h2[b], in1=h1[i % 3], op=ALU.add),
                 reads=["h1_%d" % (i % 3)], writes=["n2"])
            pre = 16 + 4 * b
            rms2(h2[b], "n2", pre, junkA, "junkA")
            P.op("dve", lambda E: E.scalar_tensor_tensor(out=h2[b], in0=h2[b], scalar=sm2[:, pre + 2:pre + 3], in1=gfb,
                                                         op0=ALU.mult, op1=ALU.mult),
                 reads=["s2_%d" % (pre + 2), "gfb"], writes=["n2"])
            P.dma("sp", lambda E: E.dma_start(out=out[i * 128:(i + 1) * 128, :], in_=h2[b]), "o%d" % b,
                  reads=["n2"])
            return nxt

        NT2 = NT if debug is None else int(debug[2:])
        prep_load(0)
        for _ in prep(0):
            pass
        if NT2 > 1:
            prep_load(1)
        pre_c = []
        for i in range(NT2):
            if i + 2 < NT2:
                prep_load(i + 2)
            hook = prep(i + 1) if i + 1 < NT2 else None
            pre_c = compute(i, hook, pre_c, i + 1 < NT2)
        P.finish([k for k in ("o0", "o1") if k in P.dsem])
        P.emit()
    return nc


def _prep_inputs(inputs):
    f = lambda a: np.ascontiguousarray(np.asarray(a, dtype=np.float32))
    x = f(inputs["x"])
    c = f(inputs["c"])
    shared = {
        "w_ada": f(inputs["w_ada"][0]),
        "b_ada": f(inputs["b_ada"][0]).reshape(1, -1),
        "g1": f(inputs["g_norm1"][0]).reshape(1, -1),
        "g2": f(inputs["g_norm2"][0]).reshape(1, -1),
        "gf": f(inputs["g_final"]).reshape(1, -1),
        "w_in": f(inputs["w_in"][0]),
        "w_co": f(inputs["w_conv_out"][0]),
        "w_so": f(inputs["w_sgu_out"][0]),
        "w_o": f(inputs["w_out"][0]),
        "w_q": f(inputs["w_query"][0]),
        "dwT": f(np.asarray(inputs["conv_dw_w"][0]).T.reshape(8, 128, 31).transpose(1, 0, 2)),
        "cvec": f(np.stack([np.asarray(inputs[k][0]).reshape(8, 128).T for k in
                            ("conv_dw_b", "conv_ln_g", "conv_ln_b")], axis=1)),
        "sgg": f(inputs["sgu_ln_g"][0]).reshape(1, -1),
        "sgb": f(inputs["sgu_ln_b"][0]).reshape(1, -1),
        "wspT": f(np.transpose(np.asarray(inputs["w_spatial"][0]), (2, 0, 1))),
        "bsp": f(inputs["b_spatial"][0]).reshape(1, -1),
        "subkT": f(np.transpose(np.asarray(inputs["sub_keys"][0]).reshape(16, 128, 128), (2, 0, 1))),
        "uv": f(np.concatenate([np.asarray(inputs["expert_u"][0]), np.asarray(inputs["expert_v"][0])], axis=1)),
    }
    in_maps = []
    for b in range(8):
        m = dict(shared)
        m["x"] = np.ascontiguousarray(x[b])
        m["c_t"] = np.ascontiguousarray(c[b].reshape(8, 128).T)
        in_maps.append(m)
    return in_maps


def kernel(**inputs):
    in_maps = _prep_inputs(inputs)
    nc = build_nc()
    res = run_bass_kernel_spmd(nc, in_maps, core_ids=list(range(8)))
    return np.stack([np.asarray(r["out"], dtype=np.float32) for r in res.results], axis=0)
```
